# Optimizing a Trainium2 kernel written in Bass

```python
import math
import jax, jax.numpy as jnp
from jax import lax
import numpy as np

D_MODEL = 1024
BATCH = 8
SEQ = 2048
DEPTH = 2

CHUNK = 64
Q_BLOCK = 128
A_Q_BLOCK = 64
EPS = 1e-6

A_HEADS = 8
A_HEAD_DIM = 64
IDX_HEADS = 8
IDX_DIM = 64
TOPK_MAX = 256

B_HEADS = 8
Q_LORA = 256
KV_LORA = 128
QK_NOPE = 64
QK_ROPE = 32
V_DIM = 64
ROPE_THETA = 10000.0

N_BUCKETS = 32
MAX_DISTANCE = 128

N_EXPERTS = 32
TOP_K = 4
D_EXPERT = 1024
SWIGLU_LIMIT = 7.0
SWIGLU_ALPHA = 1.702
MOE_BLOCK = 256

D_A = A_HEADS * A_HEAD_DIM
D_B = B_HEADS * V_DIM
D_MIX = D_A + D_B
IN_SIZES = (D_A, D_A, D_A, IDX_HEADS * IDX_DIM, IDX_DIM, IDX_HEADS, Q_LORA, KV_LORA, QK_ROPE)
D_IN = 3 * D_A + IDX_HEADS * IDX_DIM + IDX_DIM + IDX_HEADS + Q_LORA + KV_LORA + QK_ROPE

kernel_name = 'hybrid_dsa_mla_moe_adaln_trunk'


def rmsnorm(x, g):
    xf = x.astype(jnp.float32)
    y = xf * lax.rsqrt(jnp.mean(xf * xf, axis=-1, keepdims=True) + EPS)
    return (y * g).astype(x.dtype)


def rope(x, pos):
    half = x.shape[-1] // 2
    inv = ROPE_THETA ** (-jnp.arange(half, dtype=jnp.float32) / half)
    ang = pos.astype(jnp.float32)[..., None] * inv
    ang = ang.reshape(ang.shape[:2] + (1,) * (x.ndim - 3) + (half,))
    cos, sin = jnp.cos(ang), jnp.sin(ang)
    xf = x.astype(jnp.float32)
    x1, x2 = xf[..., :half], xf[..., half:]
    return jnp.concatenate([x1 * cos - x2 * sin, x2 * cos + x1 * sin], axis=-1).astype(x.dtype)


def t5_bucket(rel):
    nb = N_BUCKETS // 2
    max_exact = nb // 2
    ret = jnp.where(rel > 0, nb, 0)
    n = jnp.abs(rel)
    large = max_exact + (jnp.log(jnp.maximum(n, 1).astype(jnp.float32) / max_exact)
                         / math.log(MAX_DISTANCE / max_exact) * (nb - max_exact)).astype(jnp.int32)
    large = jnp.minimum(large, nb - 1)
    return ret + jnp.where(n < max_exact, n, large)


def to_blocks(t, qb):
    b, s = t.shape[:2]
    return jnp.moveaxis(t.reshape((b, s // qb, qb) + t.shape[2:]), 1, 0)


def from_blocks(t):
    nqb, b, qb = t.shape[:3]
    return jnp.moveaxis(t, 0, 1).reshape((b, nqb * qb) + t.shape[3:])


def indexer_sparse_attention(q_a, k_a, v_a, q_idx, k_idx, w_idx, pos, rel_bias):
    b, s = pos.shape
    topk = min(TOPK_MAX, s // 4)
    kv = jnp.concatenate([k_a, v_a], axis=-1)
    key_chunk = pos // CHUNK

    def block(args):
        q, qi, wi, pq = args
        rel_scores = jax.nn.relu(jnp.einsum('bqhd,bsd->bqhs', qi, k_idx))
        score = jnp.einsum('bqh,bqhs->bqs', wi, rel_scores).astype(jnp.float32)
        visible = key_chunk[:, None, :] <= (pq // CHUNK)[:, :, None]
        top_val, top_idx = lax.top_k(jnp.where(visible, score, -jnp.inf), topk)
        valid = jnp.isfinite(top_val)
        kv_sel = jax.vmap(lambda t, i: t[i])(kv, top_idx)
        k_sel, v_sel = jnp.split(kv_sel, 2, axis=-1)
        pos_sel = jax.vmap(lambda p, i: p[i])(pos, top_idx)
        bias = rel_bias[t5_bucket(pos_sel - pq[:, :, None])]
        logits = jnp.einsum('bqhd,bqkhd->bqkh', q, k_sel).astype(jnp.float32) * (A_HEAD_DIM ** -0.5) + bias
        p = jax.nn.softmax(jnp.where(valid[..., None], logits, -jnp.inf), axis=2)
        return jnp.einsum('bqkh,bqkhd->bqhd', p.astype(v_sel.dtype), v_sel)

    out = lax.map(block, (to_blocks(q_a, A_Q_BLOCK), to_blocks(q_idx, A_Q_BLOCK),
                          to_blocks(w_idx, A_Q_BLOCK), to_blocks(pos, A_Q_BLOCK)))
    return from_blocks(out)


def latent_attention(q_nope, q_rope, k_nope, k_rope, v, pos):
    scale = (QK_NOPE + QK_ROPE) ** -0.5
    key_chunk = pos // CHUNK

    def block(args):
        qn, qr, pq = args
        logits = (jnp.einsum('bqhd,bshd->bhqs', qn, k_nope)
                  + jnp.einsum('bqhd,bsd->bhqs', qr, k_rope)).astype(jnp.float32) * scale
        visible = key_chunk[:, None, None, :] <= (pq // CHUNK)[:, None, :, None]
        p = jax.nn.softmax(jnp.where(visible, logits, -jnp.inf), axis=-1)
        return jnp.einsum('bhqs,bshd->bqhd', p.astype(v.dtype), v)

    out = lax.map(block, (to_blocks(q_nope, Q_BLOCK), to_blocks(q_rope, Q_BLOCK), to_blocks(pos, Q_BLOCK)))
    return from_blocks(out)


def hybrid_mixer(h, pos, rel_bias, w_in, q_norm, w_uq, kv_norm, w_ukv, w_out):
    b, s, _ = h.shape
    offsets = np.cumsum(IN_SIZES)[:-1].tolist()
    proj = h @ w_in
    q_a, k_a, v_a, q_idx, k_idx, w_idx, c_q, c_kv, k_rope = jnp.split(proj, offsets, axis=-1)
    heads = lambda t, n: t.reshape(b, s, n, -1)
    y_a = indexer_sparse_attention(heads(q_a, A_HEADS), heads(k_a, A_HEADS), heads(v_a, A_HEADS),
                                   heads(q_idx, IDX_HEADS), k_idx, w_idx, pos, rel_bias)
    q = heads(rmsnorm(c_q, q_norm) @ w_uq, B_HEADS)
    kvb = heads(rmsnorm(c_kv, kv_norm) @ w_ukv, B_HEADS)
    y_b = latent_attention(q[..., :QK_NOPE], rope(q[..., QK_NOPE:], pos),
                           kvb[..., :QK_NOPE], rope(k_rope, pos), kvb[..., QK_NOPE:], pos)
    y = jnp.concatenate([y_a.reshape(b, s, D_A), y_b.reshape(b, s, D_B)], axis=-1)
    return y @ w_out


def clamped_swiglu(hid):
    x_glu, x_lin = jnp.split(hid, 2, axis=-1)
    x_glu = jnp.minimum(x_glu, SWIGLU_LIMIT)
    x_lin = jnp.clip(x_lin, -SWIGLU_LIMIT, SWIGLU_LIMIT)
    return x_glu * jax.nn.sigmoid(SWIGLU_ALPHA * x_glu) * (x_lin + 1.0)


def routed_moe(h, w_router, b_router, w1, b1, w2, b2):
    b, s, d = h.shape
    n = b * s
    xt = h.reshape(n, d)
    logits = (xt @ w_router + b_router).astype(jnp.float32)
    top_val, top_e = lax.top_k(logits, TOP_K)
    gate = jax.nn.softmax(top_val, axis=-1)
    nk = n * TOP_K
    flat_e = top_e.reshape(nk)
    flat_tok = jnp.repeat(jnp.arange(n, dtype=jnp.int32), TOP_K)
    flat_w = gate.reshape(nk)
    order = jnp.argsort(flat_e)
    se, stok, sw = flat_e[order], flat_tok[order], flat_w[order]
    counts = jnp.bincount(flat_e, length=N_EXPERTS)
    start = jnp.cumsum(counts) - counts
    padded = (counts + MOE_BLOCK - 1) // MOE_BLOCK * MOE_BLOCK
    pend = jnp.cumsum(padded)
    pstart = pend - padded
    dest = pstart[se] + (jnp.arange(nk) - start[se])
    n_blocks = -(-nk // MOE_BLOCK) + N_EXPERTS
    cap = n_blocks * MOE_BLOCK
    buf_tok = jnp.zeros((cap,), jnp.int32).at[dest].set(stok)
    buf_w = jnp.zeros((cap,), jnp.float32).at[dest].set(sw)
    block_e = jnp.minimum(jnp.searchsorted(pend, jnp.arange(n_blocks) * MOE_BLOCK, side='right'),
                          N_EXPERTS - 1)

    def expert_block(args):
        e, tok, w = args
        xe = xt[tok]
        y = clamped_swiglu(xe @ w1[e] + b1[e]) @ w2[e] + b2[e]
        return y * w[:, None]

    yb = lax.map(expert_block, (block_e, buf_tok.reshape(n_blocks, MOE_BLOCK), buf_w.reshape(n_blocks, MOE_BLOCK)))
    out = jax.ops.segment_sum(yb.reshape(cap, d), buf_tok, num_segments=n)
    return out.reshape(b, s, d).astype(h.dtype)


def setup_inputs(seed: int = 0) -> dict:
    key = jax.random.key(seed)
    ks = jax.random.split(key, 24)
    f32 = jnp.float32

    def nrm(k, shape, fan_in, mult=1.0):
        return jax.random.normal(k, shape, f32) * (mult * fan_in ** -0.5)

    x = jax.random.normal(ks[0], (BATCH, SEQ, D_MODEL), f32)
    c = jax.random.normal(ks[1], (BATCH, D_MODEL), f32)
    offset = jax.random.randint(ks[2], (BATCH, 1), 0, 64) * CHUNK
    positions = (offset + jnp.arange(SEQ, dtype=jnp.int32)[None, :]).astype(jnp.int32)
    return {
        'x': x,
        'c': c,
        'positions': positions,
        'rel_bias': 0.2 * jax.random.normal(ks[3], (N_BUCKETS, A_HEADS), f32),
        'norm_mix': 1.0 + 0.02 * jax.random.normal(ks[4], (DEPTH, D_MODEL), f32),
        'w_ada': nrm(ks[5], (DEPTH, D_MODEL, 6 * D_MODEL), D_MODEL, 0.5),
        'b_ada': 0.02 * jax.random.normal(ks[6], (DEPTH, 6 * D_MODEL), f32),
        'w_in': nrm(ks[7], (DEPTH, D_MODEL, D_IN), D_MODEL),
        'q_norm': 1.0 + 0.02 * jax.random.normal(ks[8], (DEPTH, Q_LORA), f32),
        'w_uq': nrm(ks[9], (DEPTH, Q_LORA, B_HEADS * (QK_NOPE + QK_ROPE)), Q_LORA),
        'kv_norm': 1.0 + 0.02 * jax.random.normal(ks[10], (DEPTH, KV_LORA), f32),
        'w_ukv': nrm(ks[11], (DEPTH, KV_LORA, B_HEADS * (QK_NOPE + V_DIM)), KV_LORA),
        'w_out': nrm(ks[12], (DEPTH, D_MIX, D_MODEL), D_MIX),
        'norm_ffn': 1.0 + 0.02 * jax.random.normal(ks[13], (DEPTH, D_MODEL), f32),
        'w_router': nrm(ks[14], (DEPTH, D_MODEL, N_EXPERTS), D_MODEL),
        'b_router': 0.01 * jax.random.normal(ks[15], (DEPTH, N_EXPERTS), f32),
        'w1': nrm(ks[16], (DEPTH, N_EXPERTS, D_MODEL, 2 * D_EXPERT), D_MODEL),
        'b1': 0.02 * jax.random.normal(ks[17], (DEPTH, N_EXPERTS, 2 * D_EXPERT), f32),
        'w2': nrm(ks[18], (DEPTH, N_EXPERTS, D_EXPERT, D_MODEL), D_EXPERT),
        'b2': 0.02 * jax.random.normal(ks[19], (DEPTH, N_EXPERTS, D_MODEL), f32),
        'norm_final': 1.0 + 0.02 * jax.random.normal(ks[20], (D_MODEL,), f32),
    }


def reference(x, c, positions, rel_bias, norm_mix, w_ada, b_ada, w_in, q_norm, w_uq, kv_norm, w_ukv,
              w_out, norm_ffn, w_router, b_router, w1, b1, w2, b2, norm_final):
    cond = jax.nn.silu(c)
    for l in range(DEPTH):
        mod = (cond @ w_ada[l] + b_ada[l])[:, None, :]
        sh_m, sc_m, g_m, sh_f, sc_f, g_f = jnp.split(mod, 6, axis=-1)
        h = rmsnorm(x, norm_mix[l]) * (1.0 + sc_m) + sh_m
        x = x + g_m * hybrid_mixer(h, positions, rel_bias, w_in[l], q_norm[l], w_uq[l],
                                   kv_norm[l], w_ukv[l], w_out[l])
        h = rmsnorm(x, norm_ffn[l]) * (1.0 + sc_f) + sh_f
        x = x + g_f * routed_moe(h, w_router[l], b_router[l], w1[l], b1[l], w2[l], b2[l])
    return rmsnorm(x, norm_final)
```

```python
import math
from contextlib import ExitStack
import numpy as np
import concourse.bass as bass
import concourse.mybir as mybir
from concourse.bass_utils import run_bass_kernel_spmd

F32 = mybir.dt.float32
BF16 = mybir.dt.bfloat16
I32 = mybir.dt.int32
U8 = mybir.dt.uint8
AF = mybir.ActivationFunctionType
ALU = mybir.AluOpType
AX = mybir.AxisListType

S = 2048
D = 1024
NT = 16
DEPTH = 2
NE = 32
EPS = 1e-6
TOPK = 256
NBIS = 20
BIG = 1.0e30
SC_A = 64 ** -0.5
SC_B = 96 ** -0.5


class Prog:
    NPOOL = 32

    def __init__(self, nc, es):
        self.nc = nc
        self.es = es
        self.eng = {'pe': nc.tensor, 'act': nc.scalar, 'dve': nc.vector, 'pool': nc.gpsimd, 'sp': nc.sync}
        self.sem = {e: es.enter_context(nc.semaphore("s_" + e)) for e in ('pe', 'act', 'dve', 'pool')}
        self.cnt = {e: 0 for e in self.sem}
        for k in range(self.NPOOL):
            self.sem[('d', k)] = es.enter_context(nc.semaphore(f"d_{k}"))
            self.cnt[('d', k)] = 0
        self.dnext = 0
        self.waited = {e: {} for e in self.eng}
        self.last_w = {}
        self.readers = {}
        self.nwaits = 0

    def _wait(self, e, tok):
        s, v = tok
        if s == 'pe' and e == 'pe':
            return
        if self.waited[e].get(s, 0) >= v:
            return
        self.eng[e].wait_ge(self.sem[s], v)
        self.waited[e][s] = v
        self.nwaits += 1

    def op(self, e, fn, reads=(), writes=(), dma=None):
        deps = {}
        for k in reads:
            t = self.last_w.get(k)
            if t is not None:
                deps[t[0]] = max(deps.get(t[0], 0), t[1])
        for k in writes:
            t = self.last_w.get(k)
            if t is not None:
                deps[t[0]] = max(deps.get(t[0], 0), t[1])
            for s, v in self.readers.get(k, {}).items():
                deps[s] = max(deps.get(s, 0), v)
        for s, v in deps.items():
            self._wait(e, (s, v))
        if dma is not None:
            assert e == 'sp'
            sk = ('d', self.dnext % self.NPOOL)
            self.dnext += 1
            if self.cnt[sk] > 0:
                self._wait(e, (sk, self.cnt[sk]))
            ins = fn(self.eng[e])
            ins.then_inc(self.sem[sk], 16)
            self.cnt[sk] += 16
            tok = (sk, self.cnt[sk])
        else:
            ins = fn(self.eng[e])
            ins.then_inc(self.sem[e], 1)
            self.cnt[e] += 1
            tok = (e, self.cnt[e])
        for k in writes:
            self.last_w[k] = tok
            self.readers[k] = {}
        for k in reads:
            r = self.readers.setdefault(k, {})
            r[tok[0]] = max(r.get(tok[0], 0), tok[1])
        return tok

    def wait_all_dma(self, e='sp'):
        for k in range(self.NPOOL):
            sk = ('d', k)
            if self.cnt[sk] > 0:
                self._wait(e, (sk, self.cnt[sk]))

    def barrier(self):
        for e in self.eng:
            for s in self.sem:
                if self.cnt[s] > 0:
                    self._wait(e, (s, self.cnt[s]))
        self.last_w = {}
        self.readers = {}

    def mm(self, items, reads=(), writes=()):
        def fn(pe):
            ins = None
            for (o, l, r, st, sp) in items:
                ins = pe.matmul(o, lhsT=l, rhs=r, start=st, stop=sp)
            return ins
        return self.op('pe', fn, reads, writes)

    def tr(self, items, ident, reads=(), writes=()):
        def fn(pe):
            ins = None
            for (o, i) in items:
                ins = pe.transpose(out=o, in_=i, identity=ident)
            return ins
        return self.op('pe', fn, reads, writes)


def t5_bucket_np(rel):
    nb = 16
    max_exact = 8
    ret = np.where(rel > 0, nb, 0)
    n = np.abs(rel)
    lg = np.log(np.maximum(n, 1).astype(np.float32) / np.float32(max_exact)).astype(np.float32)
    large = max_exact + (lg / np.float32(math.log(128 / max_exact)) * np.float32(nb - max_exact)).astype(np.float32).astype(np.int32)
    large = np.minimum(large, nb - 1)
    return ret + np.where(n < max_exact, n, large)


def host_consts():
    r = np.arange(128)[None, :]
    c = np.arange(128)[:, None]
    oh = np.zeros((32, 2, 128, 128), np.float32)
    for w in range(2):
        rel = (c - r) - 128 * w
        bk = t5_bucket_np(rel)
        for b in range(32):
            oh[b, w] = (bk == b)
    inv = (10000.0 ** (-np.arange(16, dtype=np.float32) / 16)).astype(np.float32)
    cols = np.zeros((128, 4), np.float32)
    for p in range(64, 96):
        i = (p - 64) % 16
        cols[p, 0] = inv[i]
        cols[p, 1] = -inv[i] if (p - 64) < 16 else inv[i]
    pw = np.zeros((128, NBIS), np.float32)
    pw[:] = 0.5 ** np.arange(1, NBIS + 1, dtype=np.float32)
    return {"oh": oh.reshape(32, 2 * 128 * 128), "ccols": cols, "pw2": pw}


class Ctx:
    pass


def build(stage=99, dbg=False):
    nc = bass.Bass("TRN2", target_bir_lowering=False)

    def dt(name, shape, dtp=F32, kind="ExternalInput"):
        return nc.dram_tensor(name, shape, dtp, kind=kind).ap()

    g = Ctx()
    g.nc = nc
    g.x_d = dt("x", [S, D]); g.c_d = dt("c", [D]); g.pos_d = dt("pos", [S], I32)
    g.relb_d = dt("rel_bias", [32, 8]); g.nmix_d = dt("norm_mix", [2, D]); g.wada_d = dt("w_ada", [2, D, 6 * D])
    g.bada_d = dt("b_ada", [2, 6 * D]); g.win_d = dt("w_in", [2, D, 2536]); g.qn_d = dt("q_norm", [2, 256])
    g.wuq_d = dt("w_uq", [2, 256, 768]); g.kvn_d = dt("kv_norm", [2, 128]); g.wukv_d = dt("w_ukv", [2, 128, 1024])
    g.wout_d = dt("w_out", [2, D, D]); g.nffn_d = dt("norm_ffn", [2, D]); g.wr_d = dt("w_router", [2, D, 32])
    g.br_d = dt("b_router", [2, 32]); g.w1_d = dt("w1", [2, 32, D, 2048]); g.b1_d = dt("b1", [2, 32, 2048])
    g.w2_d = dt("w2", [2, 32, D, D]); g.b2_d = dt("b2", [2, 32, D]); g.nfin_d = dt("norm_final", [D])
    g.oh_d = dt("oh", [32, 2 * 128 * 128]); g.ccols_d = dt("ccols", [128, 4]); g.pw2_d = dt("pw2", [128, NBIS])
    g.y_d = dt("y", [S, D], F32, "ExternalOutput")
    g.cqn_s = [dt(f"cqn_s{l}", [128, 2, S], BF16, "Internal") for l in range(2)]
    g.ckvn_s = [dt(f"ckvn_s{l}", [128, S], BF16, "Internal") for l in range(2)]
    g.krT_s = [dt(f"krT_s{l}", [32, S], BF16, "Internal") for l in range(2)]
    g.dbg = dbg
    if dbg:
        g.dbgf = dt("dbgf", [128, 32768], F32, "ExternalOutput")
        g.dbgb = dt("dbgb", [128, 65536], BF16, "ExternalOutput")

    es = ExitStack()
    P = Prog(nc, es)
    g.P = P

    def sbt(st, name, shape, dtp=F32):
        return st.enter_context(nc.sbuf_tensor("sb_" + name, shape, dtp))
    g.sbt = sbt

    g.xs = sbt(es, "xs", [128, NT, D])
    g.identb = sbt(es, "identb", [128, 128], BF16); g.identf = sbt(es, "identf", [128, 128])
    g.onesb = sbt(es, "onesb", [128, 128], BF16); g.onesf = sbt(es, "onesf", [128, 128])
    g.ccols = sbt(es, "ccols", [128, 4]); g.pw2 = sbt(es, "pw2", [128, NBIS]); g.epsc = sbt(es, "epsc", [128, 1])
    g.modc = sbt(es, "modc", [128, 2, 48])
    g.nbig = sbt(es, "nbig", [128, 1])
    g.ps = [es.enter_context(nc.psum_tensor(f"ps{i}", [128, 512], F32)) for i in range(8)]
    g.psn = 0
    import os
    g.nexp_dbg = int(os.environ.get('NEXP_DBG', NE))
    g.reserved = set()

    setup_phase(g)
    t5_tables(g, es, "t5_")
    if stage >= 1:
        for l in range(DEPTH):
            mixer_phase(g, l, stage)
            if stage >= 5:
                moe_phase(g, l, stage)
            if stage < 9:
                break
    final_phase(g, stage)
    es.close()
    return nc


def nextps(g, reserve=False):
    while True:
        b = g.psn % 8
        g.psn += 1
        if b not in g.reserved:
            break
    if reserve:
        g.reserved.add(b)
    return b


def dump(g, ap, col, key, bf=False):
    if not g.dbg:
        return
    p, n = ap.shape[0], ap.shape[1]
    d = g.dbgb if bf else g.dbgf
    g.P.op('sp', lambda e: e.dma_start(out=d[0:p, col:col + n], in_=ap), [key] if not isinstance(key, list) else key, [], dma=1)


def setup_phase(g):
    nc, P = g.nc, g.P
    for t_, kn in ((g.identb, 'identb'), (g.identf, 'identf')):
        P.op('pool', lambda e: e.memset(t_[:], 1.0), [], [kn])
        P.op('pool', lambda e: e.affine_select(out=t_[:], in_=t_[:], pattern=[[-1, 128]], compare_op=ALU.is_equal,
                                               fill=0.0, base=0, channel_multiplier=1), [kn], [kn])
    P.op('pool', lambda e: e.memset(g.onesb[:], 1.0), [], ['onesb'])
    P.op('pool', lambda e: e.memset(g.onesf[:], 1.0), [], ['onesf'])
    P.op('pool', lambda e: e.memset(g.epsc[:], EPS), [], ['epsc'])
    P.op('pool', lambda e: e.memset(g.nbig[:], -30000.0), [], ['nbig'])
    P.op('sp', lambda e: e.dma_start(out=g.ccols[:], in_=g.ccols_d[:, :]), [], ['ccols'], dma=1)
    P.op('sp', lambda e: e.dma_start(out=g.pw2[:], in_=g.pw2_d[:, :]), [], ['pw2'], dma=1)
    xv = g.x_d.rearrange("(t p) d -> p t d", p=128)
    for q in range(4):
        P.op('sp', lambda e: e.dma_start(out=g.xs[:, 4 * q:4 * q + 4, :], in_=xv[:, 4 * q:4 * q + 4, :]), [],
             [('xs', t) for t in range(4 * q, 4 * q + 4)], dma=1)

    with ExitStack() as ph:
        sb = lambda name, shape, dtp=F32: g.sbt(ph, "su_" + name, shape, dtp)
        ccol = sb("ccol", [128, 8]); cond = sb("cond", [128, 8])
        wa = [sb(f"wa{i}", [128, 8, 512]) for i in range(4)]
        brow = sb("brow", [48, 2, 128]); badaT = sb("badaT", [128, 2, 48])
        P.op('sp', lambda e: e.dma_start(out=ccol[:], in_=g.c_d.rearrange("(k p) -> p k", p=128), allow_slow_non_contiguous=True),
             [], ['ccol'], dma=1)
        P.op('act', lambda e: e.activation(out=cond[:], in_=ccol[:], func=AF.Silu), ['ccol'], ['cond'])
        for l in range(2):
            P.op('sp', lambda e: e.dma_start(out=brow[:, l, :], in_=g.bada_d[l].rearrange("(j p) -> j p", p=128)), [], ['brow'], dma=1)
        for l in range(2):
            b = nextps(g)
            P.tr([(g.ps[b][:, 0:48], brow[:, l, :])], g.identf[0:48, 0:48], ['brow', 'identf'], [('ps', b)])
            P.op('dve', lambda e: e.tensor_copy(out=badaT[:, l, :], in_=g.ps[b][:, 0:48]), [('ps', b)], ['badaT'])
        n = 0
        for l in range(2):
            wv = g.wada_d[l].rearrange("(k p) n -> p k n", p=128)
            for cg in range(12):
                s_ = n % 4
                n += 1
                P.op('sp', lambda e: e.dma_start(out=wa[s_][:], in_=wv[:, :, cg * 512:(cg + 1) * 512]), [], [('wa', s_)], dma=1)
                b = nextps(g)
                items = []
                for jc in range(4):
                    for kc in range(8):
                        items.append((g.ps[b][:, jc:jc + 1], wa[s_][:, kc, jc * 128:(jc + 1) * 128], cond[:, kc:kc + 1], kc == 0, kc == 7))
                P.mm(items, [('wa', s_), 'cond'], [('ps', b)])
                P.op('dve', lambda e: e.tensor_tensor(out=g.modc[:, l, cg * 4:cg * 4 + 4], in0=g.ps[b][:, 0:4],
                                                      in1=badaT[:, l, cg * 4:cg * 4 + 4], op=ALU.add), [('ps', b), 'badaT'], ['modc'])
        dump(g, g.modc[:].rearrange("p l j -> p (l j)"), 0, 'modc')

    P.barrier()


def rope_tables(g, st, tag):
    nc, P = g.nc, g.P
    g.cosT = g.sbt(st, tag + "cosT", [128, S], BF16); g.sinT = g.sbt(st, tag + "sinT", [128, S], BF16)
    with ExitStack() as ph:
        sb = lambda name, shape, dtp=F32: g.sbt(ph, tag + name, shape, dtp)
        posi = sb("posi", [128, S], I32); posf = sb("posf", [128, S]); ang = sb("ang", [128, S])
        nn = posi[:].bitcast(F32)
        R_ = slice(64, 96)
        P.op('sp', lambda e: e.dma_start(out=posi[R_, :], in_=g.pos_d.partition_broadcast(32)), [], ['posi'], dma=1)
        P.op('dve', lambda e: e.tensor_copy(out=posf[R_, :], in_=posi[R_, :]), ['posi'], ['posf'])
        TWO_PI = 2.0 * math.pi
        HI = 6.28125
        LO = TWO_PI - HI
        MAGIC = 12582912.0
        for which, tab, shift in ((0, g.cosT, math.pi / 2), (1, g.sinT, 0.0)):
            P.op('dve', lambda e: e.tensor_scalar(out=ang[R_, :], in0=posf[R_, :], scalar1=g.ccols[R_, which:which + 1], scalar2=shift,
                                                  op0=ALU.mult, op1=ALU.add), ['posf', 'ccols'], ['ang'])
            P.op('dve', lambda e: e.tensor_scalar(out=nn[R_, :], in0=ang[R_, :], scalar1=1.0 / TWO_PI, scalar2=MAGIC,
                                                  op0=ALU.mult, op1=ALU.add), ['ang'], ['posi'])
            P.op('dve', lambda e: e.tensor_scalar(out=nn[R_, :], in0=nn[R_, :], scalar1=MAGIC, scalar2=None, op0=ALU.subtract), ['posi'], ['posi'])
            P.op('dve', lambda e: e.scalar_tensor_tensor(out=ang[R_, :], in0=nn[R_, :], scalar=-HI, in1=ang[R_, :],
                                                         op0=ALU.mult, op1=ALU.add), ['posi', 'ang'], ['ang'])
            P.op('dve', lambda e: e.scalar_tensor_tensor(out=ang[R_, :], in0=nn[R_, :], scalar=-LO, in1=ang[R_, :],
                                                         op0=ALU.mult, op1=ALU.add), ['posi', 'ang'], ['ang'])
            P.op('dve', lambda e: e.tensor_scalar(out=ang[R_, :], in0=ang[R_, :], scalar1=math.pi, scalar2=-math.pi,
                                                  op0=ALU.min, op1=ALU.max), ['ang'], ['ang'])
            P.op('act', lambda e: e.activation(out=tab[R_, :], in_=ang[R_, :], func=AF.Sin), ['ang'], ['ropetab'])
    P.barrier()


def t5_tables(g, st, tag):
    nc, P = g.nc, g.P
    g.Tb = g.sbt(st, tag + "Tb", [128, 2, 8, 128], BF16)
    with ExitStack() as ph:
        sb = lambda name, shape, dtp=F32: g.sbt(ph, tag + name, shape, dtp)
        ohp = sb("ohp", [32, 64 * 128]); rb = sb("rb", [32, 8]); rb15 = sb("rb15", [32, 8])
        P.op('sp', lambda e: e.dma_start(out=rb[:], in_=g.relb_d[:, :]), [], ['rb'], dma=1)
        P.op('sp', lambda e: e.dma_start(out=rb15[:], in_=g.relb_d[15].partition_broadcast(32)), [], ['rb15'], dma=1)
        P.op('dve', lambda e: e.tensor_tensor(out=rb[:], in0=rb[:], in1=rb15[:], op=ALU.subtract), ['rb', 'rb15'], ['rb'])
        P.op('dve', lambda e: e.tensor_scalar(out=rb[:], in0=rb[:], scalar1=1.0 / SC_A, scalar2=None, op0=ALU.mult), ['rb'], ['rb'])
        ohv = ohp[:].rearrange("b (c r) -> b c r", c=64)
        for w in range(2):
            for cq in range(2):
                o0 = (w * 128 + cq * 64) * 128
                P.op('sp', lambda e: e.dma_start(out=ohp[:], in_=g.oh_d[:, o0:o0 + 64 * 128]), [], ['ohp'], dma=1)
                b = nextps(g)
                items = []
                for c in range(64):
                    items.append((g.ps[b][:, c * 8:(c + 1) * 8], ohv[:, c, :], rb[:], True, True))
                P.mm(items, ['ohp', 'rb'], [('ps', b)])
                P.op('dve', lambda e: e.tensor_copy(out=g.Tb[:, w, :, cq * 64:(cq + 1) * 64].rearrange("p h c -> p c h"),
                                                    in_=g.ps[b][:].rearrange("p (c h) -> p c h", h=8)), [('ps', b)], ['Tb'])
    P.barrier()


def gate_tile(g, G, col, tag, key='G'):
    nc, P = g.nc, g.P
    with ExitStack() as ph:
        dg = [g.sbt(ph, f"{tag}dg{i}", [128, 128]) for i in range(2)]
        for half in range(2):
            b = nextps(g)
            for k4 in range(4):
                kc = half * 4 + k4
                d_ = dg[kc % 2]
                P.op('dve', lambda e: e.tensor_scalar(out=d_[:], in0=g.identf[:], scalar1=col[:, kc:kc + 1], scalar2=None, op0=ALU.mult),
                     ['identf', 'modc'], [('dg', kc % 2)])
                P.mm([(g.ps[b][:, k4 * 128:(k4 + 1) * 128], g.onesf[:], d_[:], True, True)], [('dg', kc % 2), 'onesf'], [('ps', b)])
            P.op('act', lambda e: e.activation(out=G[:, half * 512:(half + 1) * 512], in_=g.ps[b][:, :], func=AF.Copy), [('ps', b)], [key])
    P.barrier()


def norm_phase(g, nw_dram, sc_col, sh_col, hT, tag):
    nc, P = g.nc, g.P
    P.barrier()
    with ExitStack() as ph:
        sb = lambda name, shape, dtp=F32: g.sbt(ph, tag + name, shape, dtp)
        nwc = sb("nwc", [128, 8]); Ac = sb("Ac", [128, 8]); ss = sb("ss", [128, NT]); rstd = sb("rstd", [128, NT])
        junk = sb("junk", [128, D], BF16)
        xn = [sb(f"xn{i}", [128, D], BF16) for i in range(2)]
        P.op('sp', lambda e: e.dma_start(out=nwc[:], in_=nw_dram.rearrange("(k p) -> p k", p=128), allow_slow_non_contiguous=True),
             [], ['nwc'], dma=1)
        P.op('dve', lambda e: e.scalar_tensor_tensor(out=Ac[:], in0=sc_col, scalar=1.0, in1=nwc[:], op0=ALU.add, op1=ALU.mult),
             ['nwc', 'modc'], ['Ac'])
        for t in range(NT):
            P.op('act', lambda e: e.activation(out=junk[:], in_=g.xs[:, t, :], func=AF.Square, accum_out=ss[:, t:t + 1]),
                 [('xs', t)], ['njunk', ('nss', t)])
        P.op('act', lambda e: e.activation(out=rstd[:], in_=ss[:], func=AF.Sqrt, scale=1.0 / D, bias=g.epsc[:, 0:1]),
             [('nss', t) for t in range(NT)] + ['epsc'], ['nrstd'])
        P.op('dve', lambda e: e.reciprocal(out=rstd[:], in_=rstd[:]), ['nrstd'], ['nrstd'])
        for t in range(NT):
            x_ = xn[t % 2]
            P.op('dve', lambda e: e.tensor_scalar(out=x_[:], in0=g.xs[:, t, :], scalar1=rstd[:, t:t + 1], scalar2=None, op0=ALU.mult),
                 [('xs', t), 'nrstd'], [('xn', t % 2)])
            b = nextps(g)
            pv = g.ps[b][:].bitcast(BF16)
            P.tr([(pv[:, kc * 128:(kc + 1) * 128], x_[:, kc * 128:(kc + 1) * 128]) for kc in range(8)], g.identb[:],
                 [('xn', t % 2), 'identb'], [('ps', b)])
            for kc in range(8):
                o = hT[:, kc, t * 128:(t + 1) * 128]
                i = pv[:, kc * 128:(kc + 1) * 128]
                if kc % 2 == 0:
                    P.op('dve', lambda e: e.tensor_scalar(out=o, in0=i, scalar1=Ac[:, kc:kc + 1], scalar2=sh_col[:, kc:kc + 1],
                                                          op0=ALU.mult, op1=ALU.add), [('ps', b), 'Ac', 'modc'], [('hT', t)])
                else:
                    P.op('act', lambda e: e.activation(out=o, in_=i, func=AF.Identity, scale=Ac[:, kc:kc + 1], bias=sh_col[:, kc:kc + 1]),
                         [('ps', b), 'Ac', 'modc'], [('hT', t)])
    P.barrier()


def load_w(g, dram_ap, stg, wbf, skey, wkey, eng='act', cols=None):
    P = g.P
    P.op('sp', lambda e: e.dma_start(out=stg, in_=dram_ap), [], [skey], dma=1)
    if eng == 'act':
        P.op('act', lambda e: e.activation(out=wbf, in_=stg, func=AF.Copy), [skey], [wkey])
    else:
        P.op(eng, lambda e: e.tensor_copy(out=wbf, in_=stg), [skey], [wkey])


def evac(g, i, out, in_, reads, writes):
    if i % 2 == 0:
        g.P.op('act', lambda e: e.activation(out=out, in_=in_, func=AF.Copy), reads, writes)
    else:
        g.P.op('dve', lambda e: e.tensor_copy(out=out, in_=in_), reads, writes)


def mixer_phase(g, l, stage):
    mixer_a(g, l, stage)
    if stage == 2 or stage >= 4:
        mixer_b(g, l, stage)
    if stage == 4 and g.dbg:
        for t in range(NT):
            dump(g, g.xs[:, t, :], t * 1024, ('xs', t))


def make_getw(g, wv, stg, wbf):
    nw = [0]
    nslot = len(stg)

    def getw(lo, n):
        s_ = nw[0] % nslot
        nw[0] += 1
        load_w(g, wv[:, :, lo:lo + n], stg[s_][:, :, 0:n], wbf[s_][:, :, 0:n], ('stg', s_), ('wbf', s_),
               eng=('act' if s_ == 0 else 'dve'))
        return s_
    return getw


def mixer_a(g, l, stage):
    nc, P = g.nc, g.P
    P.barrier()
    mc = g.modc
    allh = [('hT', t) for t in range(NT)]
    with ExitStack() as mx:
        sbm = lambda name, shape, dtp=F32: g.sbt(mx, f"m{l}_" + name, shape, dtp)
        kTa = sbm("kTa", [128, 4, S], BF16); qTa = sbm("qTa", [128, 4, S], BF16); qiT = sbm("qiT", [128, 4, S], BF16)
        kiT = sbm("kiT", [128, S], BF16); va = sbm("va", [128, NT, 8, 64], BF16); widx = sbm("widx", [128, NT, 8])
        with ExitStack() as pj:
            sb = lambda name, shape, dtp=F32: g.sbt(pj, f"p{l}_" + name, shape, dtp)
            hT = sb("hT", [128, 8, S], BF16)
            norm_phase(g, g.nmix_d[l], mc[:, l, 8:16], mc[:, l, 0:8], hT, f"n{l}a_")
            if stage == 1:
                for kc in range(8):
                    dump(g, hT[:, kc, :], kc * 2048, allh, bf=True)
                return
            pa = ExitStack()
            sa = lambda name, shape, dtp=F32: g.sbt(pa, f"pa{l}_" + name, shape, dtp)
            stg = [sa(f"stg{i}", [128, 8, 128]) for i in range(4)]
            wbf = [sa(f"wbf{i}", [128, 8, 128], BF16) for i in range(4)]
            wki = sa("wki", [128, 8, 128], BF16)
            getw = make_getw(g, g.win_d[l].rearrange("(k p) n -> p k n", p=128), stg, wbf)

            def fm_proj(s_, c0, m, dst_fn, tag_):
                for tg in range(4):
                    b = nextps(g)
                    P.mm([(g.ps[b][0:m, :], wbf[s_][:, kc, c0:c0 + m], hT[:, kc, tg * 512:(tg + 1) * 512], kc == 0, kc == 7)
                          for kc in range(8)], [('wbf', s_)] + allh[4 * tg:4 * tg + 4], [('ps', b)])
                    evac(g, tg, dst_fn(tg), g.ps[b][0:m, :], [('ps', b)], [tag_])

            for (dst, c_lo, kn) in ((qTa, 0, 'qTa'), (kTa, 512, 'kTa'), (qiT, 1536, 'qiT')):
                slots = [getw(c_lo + fch * 128, 128) for fch in range(4)]
                for fch in range(4):
                    s_ = slots[fch]
                    fm_proj(s_, 0, 128, lambda tg: dst[:, fch, tg * 512:(tg + 1) * 512], kn)
            vslots = [getw(1024 + hp * 128, 128) for hp in range(4)]
            for hp in range(4):
                s_ = vslots[hp]
                for t in range(NT):
                    b = nextps(g)
                    P.mm([(g.ps[b][:, 0:128], hT[:, kc, t * 128:(t + 1) * 128], wbf[s_][:, kc, 0:128], kc == 0, kc == 7) for kc in range(8)],
                         [('wbf', s_), ('hT', t)], [('ps', b)])
                    evac(g, t, va[:, t, hp * 2:hp * 2 + 2, 0:64], g.ps[b][:, 0:128].rearrange("p (h d) -> p h d", h=2),
                         [('ps', b)], [('va', t)])
            s_ = getw(2048, 72)
            P.op('dve', lambda e: e.tensor_copy(out=wki[:, :, 0:64], in_=stg[s_][:, :, 0:64]), [('stg', s_)], ['wki'])
            P.op('dve', lambda e: e.tensor_copy(out=wki[:, :, 64:128], in_=stg[s_][:, :, 0:64]), [('stg', s_)], ['wki'])
            for tg in range(4):
                b = nextps(g)
                P.mm([(g.ps[b][:, :], wki[:, kc, :], hT[:, kc, tg * 512:(tg + 1) * 512], kc == 0, kc == 7) for kc in range(8)],
                     ['wki'] + allh[4 * tg:4 * tg + 4], [('ps', b)])
                evac(g, tg, kiT[:, tg * 512:(tg + 1) * 512], g.ps[b][:, :], [('ps', b)], ['kiT'])
            for t in range(NT):
                b = nextps(g)
                P.mm([(g.ps[b][:, 0:8], hT[:, kc, t * 128:(t + 1) * 128], wbf[s_][:, kc, 64:72], kc == 0, kc == 7) for kc in range(8)],
                     [('wbf', s_), ('hT', t)], [('ps', b)])
                evac(g, t, widx[:, t, :], g.ps[b][:, 0:8], [('ps', b)], ['widx'])
            P.barrier()
            pa.close()
            b_small_proj(g, l, hT)
            if stage == 2:
                o = 0
                for (tn, kn, nch) in ((qTa, 'qTa', 4), (kTa, 'kTa', 4), (qiT, 'qiT', 4)):
                    for c_ in range(nch):
                        dump(g, tn[:, c_, :], o, kn, bf=True); o += 2048
                dump(g, kiT[:], o, 'kiT', bf=True); o += 2048
                dump(g, va[:].rearrange("p t h d -> p (t h d)"), o, [('va', t) for t in range(NT)], bf=True); o += NT * 8 * 64
                dump(g, widx[:].rearrange("p t h -> p (t h)"), 0, 'widx')
        if stage == 2:
            return
        attn_a_phase(g, l, stage, kTa, qTa, qiT, kiT, va, widx)


def attn_core(g, I, h, lhs_k, rhs_q, v_ap, scale, bias_items, maskT, diag_zero, E, Pm, bO, bD, cnt, nqt=4):
    P = g.P
    W = nqt * 128
    jmax = nqt * I + nqt - 1
    hs = slice((h % 2) * 64, (h % 2) * 64 + 64)
    DEPTH_ = 2
    pend = []

    def front(j):
        qlo = max(0, j - nqt * I) * 128
        nq = W - qlo
        bS = nextps(g)
        items = [(g.ps[bS][:, qlo:W], lhs_k(j), rhs_q(qlo), True, True)]
        if bias_items is not None:
            items = [(g.ps[bS][:, qlo:W], lhs_k(j), rhs_q(qlo), True, False)]
            for i in (j, j + 1):
                if nqt * I <= i <= nqt * I + nqt - 1:
                    c0 = (i - nqt * I) * 128
                    items.append((g.ps[bS][:, c0:c0 + 128], bias_items(i - j), g.identb[:], False, False))
            items.append((g.ps[bS][:, qlo:W], g.identb[:], maskT[:, j, qlo:W], False, True))
        P.mm(items, ['attn_in', ('maskT', I % 2)] if maskT is not None else ['attn_in'], [('ps', bS)])
        k = cnt[0] % len(E)
        cnt[0] += 1
        P.op('act', lambda e: e.activation(out=E[k][:, 0:nq], in_=g.ps[bS][:, qlo:W], func=AF.Exp, scale=scale), [('ps', bS)], [('E', k)])
        if diag_zero and j >= nqt * I:
            P.op('pool', lambda e: e.memset(E[k][64:128, 0:64], 0.0), [], [('E', k)])
        rhs, rk = E[k][:, 0:nq], ('E', k)
        return (j, qlo, rhs, rk)

    def back(j, qlo, rhs, rk):
        P.mm([(g.ps[bO][hs, qlo:W], v_ap(j), rhs, j == 0, j == jmax),
              (g.ps[bD][hs, qlo:W], g.onesb[:, 0:64], rhs, j == 0, j == jmax)], [rk, 'attn_in', 'onesb'], [('ps', bO), ('ps', bD)])

    for j in range(jmax + 1):
        pend.append(front(j))
        if len(pend) > DEPTH_:
            back(*pend.pop(0))
    while pend:
        back(*pend.pop(0))


def attn_finish(g, l, I, npair_done, yT, rden, bO, bD, pr, W=512):
    P = g.P
    P.op('dve', lambda e: e.reciprocal(out=rden[:, 0:W], in_=g.ps[bD][:, 0:W]), [('ps', bD)], ['rden'])
    P.op('dve', lambda e: e.tensor_tensor(out=yT[:, pr, 0:W], in0=g.ps[bO][:, 0:W], in1=rden[:, 0:W], op=ALU.mult), [('ps', bO), 'rden'], ['yT'])


def load_wo(g, l, half, wo, wostg, Gm):
    P = g.P
    for pr in range(4):
        r0 = half * 512 + pr * 128
        P.op('sp', lambda e: e.dma_start(out=wostg[:], in_=g.wout_d[l][r0:r0 + 128, :]), [], ['wostg'], dma=1)
        P.op('dve' if pr % 2 else 'pool', lambda e: e.tensor_tensor(out=wo[:, pr, :], in0=wostg[:], in1=Gm[:], op=ALU.mult), ['wostg', 'G'], ['wo'])


def out_proj(g, I, yT, wo, nqt=4):
    P = g.P
    for tq in range(nqt):
        t = nqt * I + tq
        for dh in range(2):
            b = nextps(g)
            P.mm([(g.ps[b][:, :], yT[:, pr, tq * 128:(tq + 1) * 128], wo[:, pr, dh * 512:(dh + 1) * 512], pr == 0, pr == 3) for pr in range(4)],
                 ['yT', 'wo'], [('ps', b)])
            P.op('dve', lambda e: e.tensor_tensor(out=g.xs[:, t, dh * 512:(dh + 1) * 512], in0=g.ps[b][:, :],
                                                  in1=g.xs[:, t, dh * 512:(dh + 1) * 512], op=ALU.add), [('ps', b), ('xs', t)], [('xs', t)])


MASKNEG = 30000.0


def attn_a_phase(g, l, stage, kTa, qTa, qiT, kiT, va, widx):
    nc, P = g.nc, g.P
    P.barrier()
    with ExitStack() as at:
        sb = lambda name, shape, dtp=F32: g.sbt(at, f"a{l}_" + name, shape, dtp)
        sc = sb("sc", [128, 2, S]); Rr = [sb(f"R{i}", [128, 512], BF16) for i in range(3)]
        Dh = [sb(f"Dh{i}", [128, 8, 128], BF16) for i in range(2)]; selb = [sb(f"selb{i}", [128, S], BF16) for i in range(2)]
        maskT2 = sb("maskT", [128, 2, NT, 256], BF16)
        lo = sb("lo", [128, 2]); hi = sb("hi", [128, 2]); w0 = sb("w0", [128, 1]); steps = sb("steps", [128, NBIS])
        mid = sb("mid", [128, 2]); cntc = sb("cntc", [128, 2]); tmp = sb("tmp", [128, 2])
        E = [sb(f"E{i}", [128, 512], BF16) for i in range(4)]
        yT = sb("yT", [128, 4, 512], BF16); rden = sb("rden", [128, 512])
        wo = sb("wo", [128, 4, D], BF16)
        Gm = selb[1][:].bitcast(F32)
        wostg = selb[0][:].bitcast(F32)
        gate_tile(g, Gm, g.modc[:, l, 16:24], f"gm{l}a_", key=('selb', 1))
        for pr in range(4):
            r0 = pr * 128
            P.op('sp', lambda e: e.dma_start(out=wostg, in_=g.wout_d[l][r0:r0 + 128, :]), [], [('selb', 0)], dma=1)
            P.op('dve' if pr % 2 else 'pool', lambda e: e.tensor_tensor(out=wo[:, pr, :], in0=wostg, in1=Gm, op=ALU.mult),
                 [('selb', 0), ('selb', 1)], ['wo'])
        P.op('pool', lambda e: e.memset(maskT2[:], 0.0), [], [('maskT', 0), ('maskT', 1)])
        ecnt = [0]

        def static_pair(I, pair):
            maskT = maskT2[:, I % 2]
            for i in pair:
                qoff = (i - 2 * I) * 128
                P.op('pool', lambda e: e.memset(maskT[:, 0:i + 1, qoff:qoff + 128], 0.0), [], [('maskT', I % 2)])
                P.op('pool', lambda e: e.memset(maskT[64:128, i, qoff:qoff + 64], -MASKNEG), [], [('maskT', I % 2)])

        def scores_bisect(I, pair):
            for idx, i in enumerate(pair):
                ncols = (i + 1) * 128
                for h in range(8):
                    P.op('act', lambda e: e.activation(out=Dh[idx][:, h, :], in_=g.identb[:], func=AF.Copy, scale=widx[:, i, h:h + 1]),
                         ['identb', 'widx'], [('Dh', idx)])
                ncg = (ncols + 511) // 512
                for cg in range(ncg):
                    w = min(512, ncols - cg * 512)
                    bS = nextps(g, True)
                    pend = []
                    for h in range(8 + 2):
                        if h < 8:
                            hs = slice((h % 2) * 64, (h % 2) * 64 + 64)
                            bX = nextps(g)
                            r_ = h % 3
                            P.mm([(g.ps[bX][:, 0:w], qiT[hs, h // 2, i * 128:(i + 1) * 128], kiT[hs, cg * 512:cg * 512 + w], True, True)],
                                 ['qiT', 'kiT'], [('ps', bX)])
                            P.op('act', lambda e: e.activation(out=Rr[r_][:, 0:w], in_=g.ps[bX][:, 0:w], func=AF.Relu), [('ps', bX)], [('R', r_)])
                            pend.append((h, r_))
                        if h >= 2:
                            hh, rr = pend.pop(0)
                            P.mm([(g.ps[bS][:, 0:w], Dh[idx][:, hh, :], Rr[rr][:, 0:w], hh == 0, hh == 7)], [('Dh', idx), ('R', rr)], [('ps', bS)])
                    P.op('dve', lambda e: e.tensor_copy(out=sc[:, idx, cg * 512:cg * 512 + w], in_=g.ps[bS][:, 0:w]), [('ps', bS)], [('sc', idx)])
                    g.reserved.discard(bS)
                P.op('pool', lambda e: e.memset(sc[0:64, idx, ncols - 64:ncols], -BIG), [], [('sc', idx)])
                P.op('dve', lambda e: e.tensor_reduce(out=hi[:, idx:idx + 1], in_=sc[:, idx, 0:ncols], axis=AX.X, op=ALU.max), [('sc', idx)], ['hi'])
                P.op('dve', lambda e: e.tensor_reduce(out=lo[:, idx:idx + 1], in_=sc[:, idx, 0:ncols - 64], axis=AX.X, op=ALU.min), [('sc', idx)], ['lo'])
            P.op('dve', lambda e: e.tensor_tensor(out=hi[:], in0=hi[:], in1=lo[:], op=ALU.subtract), ['hi', 'lo'], ['hi'])
            P.op('dve', lambda e: e.tensor_tensor(out=w0[:], in0=hi[:, 0:1], in1=hi[:, 1:2], op=ALU.max), ['hi'], ['w0'])
            P.op('dve', lambda e: e.tensor_scalar(out=steps[:], in0=g.pw2[:], scalar1=w0[:, 0:1], scalar2=None, op0=ALU.mult),
                 ['pw2', 'w0'], ['steps'])
            P.op('dve', lambda e: e.tensor_scalar(out=mid[:], in0=lo[:], scalar1=steps[:, 0:1], scalar2=None, op0=ALU.add), ['lo', 'steps'], ['mid'])
            for k in range(NBIS):
                for idx, i in enumerate(pair):
                    ncols = (i + 1) * 128
                    P.op('dve', lambda e: e.tensor_scalar(out=selb[idx][:, 0:ncols], in0=sc[:, idx, 0:ncols], scalar1=mid[:, idx:idx + 1], scalar2=None,
                                                          op0=ALU.is_ge, op1=ALU.add, accum_out=cntc[:, idx:idx + 1]),
                         [('sc', idx), 'mid'], [('selb', idx), ('cntc', idx)])
                P.op('dve', lambda e: e.tensor_scalar(out=tmp[:], in0=cntc[:], scalar1=TOPK - 0.5, scalar2=steps[:, k:k + 1],
                                                      op0=ALU.is_ge, op1=ALU.mult), [('cntc', 0), ('cntc', 1), 'steps'], ['tmp'])
                if k + 1 < NBIS:
                    P.op('dve', lambda e: e.scalar_tensor_tensor(out=mid[:], in0=tmp[:], scalar=steps[:, k + 1:k + 2], in1=mid[:],
                                                                 op0=ALU.subtract, op1=ALU.add), ['tmp', 'steps', 'mid'], ['mid'])
                else:
                    P.op('dve', lambda e: e.scalar_tensor_tensor(out=lo[:], in0=mid[:], scalar=steps[:, k:k + 1], in1=tmp[:],
                                                                 op0=ALU.subtract, op1=ALU.add), ['tmp', 'steps', 'mid'], ['lo'])
            for idx, i in enumerate(pair):
                ncols = (i + 1) * 128
                P.op('dve', lambda e: e.tensor_scalar(out=selb[idx][:, 0:ncols], in0=sc[:, idx, 0:ncols], scalar1=lo[:, idx:idx + 1], scalar2=None,
                                                      op0=ALU.is_ge), [('sc', idx), 'lo'], [('selb', idx)])

        def finalize(I, pair):
            maskT = maskT2[:, I % 2]
            for idx, i in enumerate(pair):
                qoff = (i - 2 * I) * 128
                for jb in range(0, i + 1, 8):
                    n = min(8, i + 1 - jb)
                    b = nextps(g)
                    pv = g.ps[b][:].bitcast(BF16)
                    P.tr([(pv[:, jj * 128:(jj + 1) * 128], selb[idx][:, (jb + jj) * 128:(jb + jj + 1) * 128]) for jj in range(n)], g.identb[:],
                         [('selb', idx), 'identb'], [('ps', b)])
                    P.op('act', lambda e: e.activation(out=maskT[:, jb:jb + n, qoff:qoff + 128],
                                                       in_=pv[:, 0:n * 128].rearrange("p (j q) -> p j q", j=n), func=AF.Identity,
                                                       scale=MASKNEG, bias=g.nbig[:, 0:1]), [('ps', b), 'nbig'], [('maskT', I % 2)])

        def attend(I, prs):
            maskT = maskT2[:, I % 2]
            for pr in prs:
                bO = nextps(g, True); bD = nextps(g, True)
                for h in (2 * pr, 2 * pr + 1):
                    hs = slice((h % 2) * 64, (h % 2) * 64 + 64)
                    attn_core(g, I, h,
                              lambda j: kTa[hs, pr, j * 128:(j + 1) * 128],
                              lambda qlo: qTa[hs, pr, I * 256 + qlo:(I + 1) * 256],
                              lambda j: va[:, j, h, :], SC_A,
                              lambda w_: g.Tb[:, w_, h, :], maskT, False, E, None, bO, bD, ecnt, nqt=2)
                attn_finish(g, l, I, pr, yT, rden, bO, bD, pr, W=256)
                g.reserved.discard(bO); g.reserved.discard(bD)

        static_pair(0, (0, 1))
        scores_bisect(1, (2, 3)); finalize(1, (2, 3))
        for p in range(8):
            nxt = p + 2 < 8
            attend(p, (0, 1))
            if nxt:
                scores_bisect(p + 2, (2 * p + 4, 2 * p + 5))
            attend(p, (2, 3))
            if stage == 3:
                for pr in range(4):
                    dump(g, yT[:, pr, 0:256], 16384 + (p * 4 + pr) * 256, 'yT', bf=True)
            out_proj(g, p, yT, wo, nqt=2)
            if nxt:
                finalize(p + 2, (2 * p + 4, 2 * p + 5))
    P.barrier()


def b_small_proj(g, l, hT):
    nc, P = g.nc, g.P
    allh = [('hT', t) for t in range(NT)]
    R_ = slice(64, 96)
    P.barrier()
    with ExitStack() as pj:
        sb = lambda name, shape, dtp=F32: g.sbt(pj, f"pb{l}_" + name, shape, dtp)
        rope_tables(g, pj, f"rta{l}_")
        stg = [sb(f"stg{i}", [128, 8, 128]) for i in range(2)]
        wbf = [sb(f"wbf{i}", [128, 8, 128], BF16) for i in range(2)]
        wkr = sb("wkr", [128, 8, 96], BF16); wkrS = sb("wkrS", [128, 8, 96], BF16)
        qnc = sb("qnc", [128, 2]); kvnc = sb("kvnc", [128, 1])
        sq = [sb(f"sq{i}", [128, 512], BF16) for i in range(2)]
        cqr = sb("cqr", [128, 2, 512], BF16); r1 = sb("r1", [128, 512]); r2 = sb("r2", [128, 512])
        t1 = r1; t2 = r2
        oq = [sb("oq0", [128, 2, 512], BF16)] * 2
        okv = [sb("okv0", [128, 512], BF16)] * 2
        okr = [sb("okr0", [128, 512], BF16)] * 2
        getw = make_getw(g, g.win_d[l].rearrange("(k p) n -> p k n", p=128), stg, wbf)
        P.op('sp', lambda e: e.dma_start(out=qnc[:], in_=g.qn_d[l].rearrange("(k p) -> p k", p=128), allow_slow_non_contiguous=True),
             [], ['qnc'], dma=1)
        P.op('sp', lambda e: e.dma_start(out=kvnc[:], in_=g.kvn_d[l].rearrange("(k p) -> p k", p=128), allow_slow_non_contiguous=True),
             [], ['kvnc'], dma=1)
        sl = [getw(2120, 128), getw(2248, 128)]
        for tg in range(4):
            tsl = slice(tg * 512, (tg + 1) * 512)
            bs = nextps(g, True)
            for fch in range(2):
                b = nextps(g)
                P.mm([(g.ps[b][:, :], wbf[sl[fch]][:, kc, :], hT[:, kc, tsl], kc == 0, kc == 7)
                      for kc in range(8)], [('wbf', sl[fch])] + allh[4 * tg:4 * tg + 4], [('ps', b)])
                P.op('act', lambda e: e.activation(out=cqr[:, fch, :], in_=g.ps[b][:, :], func=AF.Copy), [('ps', b)], [('cqr', fch)])
                P.op('act', lambda e: e.activation(out=sq[fch][:], in_=g.ps[b][:, :], func=AF.Square), [('ps', b)], [('sq', fch)])
            P.mm([(g.ps[bs][:, :], g.onesb[:], sq[fch][:], fch == 0, fch == 1) for fch in range(2)],
                 [('sq', 0), ('sq', 1), 'onesb'], [('ps', bs)])
            g.reserved.discard(bs)
            P.op('act', lambda e: e.activation(out=r1[:], in_=g.ps[bs][:, :], func=AF.Sqrt, scale=1.0 / 256, bias=g.epsc[:, 0:1]),
                 [('ps', bs), 'epsc'], ['r1'])
            P.op('dve', lambda e: e.reciprocal(out=r2[:], in_=r1[:]), ['r1'], ['r2'])
            for fch in range(2):
                P.op('dve', lambda e: e.scalar_tensor_tensor(out=oq[tg % 2][:, fch, :], in0=cqr[:, fch, :],
                                                             scalar=qnc[:, fch:fch + 1], in1=r2[:], op0=ALU.mult, op1=ALU.mult),
                     [('cqr', fch), 'qnc', 'r2'], [('oq', 0)])
            P.op('sp', lambda e: e.dma_start(out=g.cqn_s[l][:, :, tsl], in_=oq[tg % 2][:]), [('oq', 0)], [], dma=1)
        s_ = getw(2376, 128)
        s2 = getw(2504, 32)
        P.op('pool', lambda e: e.memset(wkr[:], 0.0), [], ['wkr'])
        P.op('pool', lambda e: e.memset(wkrS[:], 0.0), [], ['wkrS'])
        P.op('dve', lambda e: e.tensor_copy(out=wkr[:, :, 64:96], in_=stg[s2][:, :, 0:32]), [('stg', s2)], ['wkr'])
        P.op('dve', lambda e: e.tensor_copy(out=wkrS[:, :, 64:80], in_=stg[s2][:, :, 16:32]), [('stg', s2)], ['wkrS'])
        P.op('dve', lambda e: e.tensor_copy(out=wkrS[:, :, 80:96], in_=stg[s2][:, :, 0:16]), [('stg', s2)], ['wkrS'])
        for tg in range(4):
            b = nextps(g); bs = nextps(g)
            tsl = slice(tg * 512, (tg + 1) * 512)
            P.mm([(g.ps[b][:, :], wbf[s_][:, kc, 0:128], hT[:, kc, tsl], kc == 0, kc == 7) for kc in range(8)],
                 [('wbf', s_)] + allh[4 * tg:4 * tg + 4], [('ps', b)])
            P.op('act', lambda e: e.activation(out=cqr[:, 0, :], in_=g.ps[b][:, :], func=AF.Copy), [('ps', b)], [('cqr', 0)])
            P.op('act', lambda e: e.activation(out=sq[0][:], in_=g.ps[b][:, :], func=AF.Square), [('ps', b)], [('sq', 0)])
            P.mm([(g.ps[bs][:, :], g.onesb[:], sq[0][:], True, True)], [('sq', 0), 'onesb'], [('ps', bs)])
            P.op('act', lambda e: e.activation(out=r1[:], in_=g.ps[bs][:, :], func=AF.Sqrt, scale=1.0 / 128, bias=g.epsc[:, 0:1]),
                 [('ps', bs), 'epsc'], ['r1'])
            P.op('dve', lambda e: e.reciprocal(out=r2[:], in_=r1[:]), ['r1'], ['r2'])
            P.op('dve', lambda e: e.scalar_tensor_tensor(out=okv[tg % 2][:], in0=cqr[:, 0, :], scalar=kvnc[:, 0:1], in1=r2[:],
                                                         op0=ALU.mult, op1=ALU.mult), [('cqr', 0), 'kvnc', 'r2'], [('okv', 0)])
            P.op('sp', lambda e: e.dma_start(out=g.ckvn_s[l][:, tsl], in_=okv[tg % 2][:]), [('okv', 0)], [], dma=1)
            ba = nextps(g); bb = nextps(g)
            P.mm([(g.ps[ba][0:96, :], wkr[:, kc, :], hT[:, kc, tsl], kc == 0, kc == 7) for kc in range(8)],
                 ['wkr'] + allh[4 * tg:4 * tg + 4], [('ps', ba)])
            P.mm([(g.ps[bb][0:96, :], wkrS[:, kc, :], hT[:, kc, tsl], kc == 0, kc == 7) for kc in range(8)],
                 ['wkrS'] + allh[4 * tg:4 * tg + 4], [('ps', bb)])
            P.op('dve', lambda e: e.tensor_tensor(out=t1[R_, :], in0=g.ps[ba][R_, :], in1=g.cosT[R_, tsl], op=ALU.mult),
                 [('ps', ba), 'ropetab'], ['r1'])
            P.op('dve', lambda e: e.tensor_tensor(out=t2[R_, :], in0=g.ps[bb][R_, :], in1=g.sinT[R_, tsl], op=ALU.mult),
                 [('ps', bb), 'ropetab'], ['r2'])
            P.op('pool', lambda e: e.tensor_tensor(out=okr[tg % 2][R_, :], in0=t1[R_, :], in1=t2[R_, :], op=ALU.add), ['r1', 'r2'], [('okr', 0)])
            P.op('sp', lambda e: e.dma_start(out=g.krT_s[l][:, tsl], in_=okr[tg % 2][R_, :]), [('okr', 0)], [], dma=1)
    P.barrier()


def mixer_b(g, l, stage):
    nc, P = g.nc, g.P
    P.barrier()
    R_ = slice(64, 96)
    with ExitStack() as mx:
        sbm = lambda name, shape, dtp=F32: g.sbt(mx, f"mb{l}_" + name, shape, dtp)
        cqn = sbm("cqn", [128, 2, S], BF16); ckvn = sbm("ckvn", [128, S], BF16); krT = sbm("krT", [128, S], BF16)
        rope_tables(g, mx, f"rt{l}_")
        P.op('sp', lambda e: e.dma_start(out=cqn[:], in_=g.cqn_s[l][:, :, :]), [], ['cqn'], dma=1)
        P.op('sp', lambda e: e.dma_start(out=ckvn[:], in_=g.ckvn_s[l][:, :]), [], ['ckvn'], dma=1)
        P.op('sp', lambda e: e.dma_start(out=krT[R_, :], in_=g.krT_s[l][:, :]), [], ['krT'], dma=1)
        if stage == 2:
            o = 12 * 2048 + 2048 + NT * 8 * 64
            for c_ in range(2):
                dump(g, cqn[:, c_, :], o, 'cqn', bf=True); o += 2048
            dump(g, ckvn[:], o, 'ckvn', bf=True); o += 2048
            dump(g, krT[R_, :], o, 'krT', bf=True); o += 2048
            return
        attn_b_phase(g, l, stage, cqn, ckvn, krT)


def attn_b_phase(g, l, stage, cqn, ckvn, krT):
    nc, P = g.nc, g.P
    P.barrier()
    R_ = slice(64, 96)
    with ExitStack() as at:
        sb = lambda name, shape, dtp=F32: g.sbt(at, f"b{l}_" + name, shape, dtp)
        qTb = sb("qTb", [96, 8, S], BF16); kTb = sb("kTb", [96, 8, S], BF16); Vb = sb("Vb", [128, NT, 8, 64], BF16)
        with ExitStack() as up:
            su = lambda name, shape, dtp=F32: g.sbt(up, f"u{l}_" + name, shape, dtp)
            wuqst = su("wuqst", [128, 2, 768]); wuq = su("wuq", [128, 2, 8, 96], BF16); wuqS = su("wuqS", [128, 2, 8, 96], BF16)
            wukvst = su("wukvst", [128, 1024]); wukv = su("wukv", [128, 1024], BF16)
            t1 = su("t1", [128, 512]); t2 = su("t2", [128, 512])
            P.op('sp', lambda e: e.dma_start(out=wuqst[:], in_=g.wuq_d[l].rearrange("(k p) n -> p k n", p=128)), [], ['wuqst'], dma=1)
            P.op('sp', lambda e: e.dma_start(out=wukvst[:], in_=g.wukv_d[l][:, :]), [], ['wukvst'], dma=1)
            wv4 = wuqst[:].rearrange("p k (h f) -> p k h f", h=8)
            P.op('act', lambda e: e.activation(out=wuq[:], in_=wv4, func=AF.Copy), ['wuqst'], ['wuq'])
            P.op('dve', lambda e: e.tensor_copy(out=wuqS[:, :, :, 0:64], in_=wv4[:, :, :, 0:64]), ['wuqst'], ['wuqS'])
            P.op('dve', lambda e: e.tensor_copy(out=wuqS[:, :, :, 64:80], in_=wv4[:, :, :, 80:96]), ['wuqst'], ['wuqS'])
            P.op('dve', lambda e: e.tensor_copy(out=wuqS[:, :, :, 80:96], in_=wv4[:, :, :, 64:80]), ['wuqst'], ['wuqS'])
            P.op('act', lambda e: e.activation(out=wukv[:], in_=wukvst[:], func=AF.Copy), ['wukvst'], ['wukv'])
            n = 0
            for h in range(8):
                for tg in range(4):
                    tsl = slice(tg * 512, (tg + 1) * 512)
                    bA = nextps(g); bB = nextps(g); bK = nextps(g)
                    P.mm([(g.ps[bA][0:96, :], wuq[:, kc, h, :], cqn[:, kc, tsl], kc == 0, kc == 1) for kc in range(2)], ['wuq', 'cqn'], [('ps', bA)])
                    P.mm([(g.ps[bB][0:96, :], wuqS[:, kc, h, :], cqn[:, kc, tsl], kc == 0, kc == 1) for kc in range(2)], ['wuqS', 'cqn'], [('ps', bB)])
                    P.mm([(g.ps[bK][0:64, :], wukv[:, h * 128:h * 128 + 64], ckvn[:, tsl], True, True)], ['wukv', 'ckvn'], [('ps', bK)])
                    P.op('act', lambda e: e.activation(out=qTb[0:64, h, tsl], in_=g.ps[bA][0:64, :], func=AF.Copy), [('ps', bA)], ['qTb'])
                    P.op('dve', lambda e: e.tensor_tensor(out=t1[R_, :], in0=g.ps[bA][R_, :], in1=g.cosT[R_, tsl], op=ALU.mult),
                         [('ps', bA), 'ropetab'], ['t1'])
                    P.op('dve', lambda e: e.tensor_tensor(out=t2[R_, :], in0=g.ps[bB][R_, :], in1=g.sinT[R_, tsl], op=ALU.mult),
                         [('ps', bB), 'ropetab'], ['t2'])
                    P.op('pool', lambda e: e.tensor_tensor(out=qTb[R_, h, tsl], in0=t1[R_, :], in1=t2[R_, :], op=ALU.add), ['t1', 't2'], ['qTb'])
                    evac(g, n, kTb[0:64, h, tsl], g.ps[bK][0:64, :], [('ps', bK)], ['kTb'])
                    n += 1
                P.op('pool', lambda e: e.tensor_copy(out=kTb[R_, h, :], in_=krT[R_, :]), ['krT'], ['kTb'])
            wvv = wukv[:].rearrange("p (h t d) -> p h t d", h=8, t=2)[:, :, 1, :]
            for t in range(NT):
                b = nextps(g)
                P.mm([(g.ps[b][:, :], ckvn[:, t * 128:(t + 1) * 128], wvv, True, True)], ['wukv', 'ckvn'], [('ps', b)])
                evac(g, t, Vb[:, t, :, :], g.ps[b][:, :].rearrange("p (h d) -> p h d", h=8), [('ps', b)], ['Vb'])
            if stage == 4 and g.dbg:
                for h in range(8):
                    dump(g, qTb[:, h, :], h * 2048, 'qTb', bf=True)
                    dump(g, kTb[:, h, :], 16384 + h * 2048, 'kTb', bf=True)
        P.barrier()
        Gm = sb("Gm", [128, D])
        gate_tile(g, Gm, g.modc[:, l, 16:24], f"gm{l}b_")
        E = [sb(f"E{i}", [128, 512], BF16) for i in range(4)]
        yT = sb("yT", [128, 4, 512], BF16); rden = sb("rden", [128, 512])
        wo = sb("wo", [128, 4, D], BF16); wostg = sb("wostg", [128, D])
        load_wo(g, l, 1, wo, wostg, Gm)
        ecnt = [0]
        for I in range(4):
            for pr in range(4):
                bO = nextps(g, True); bD = nextps(g, True)
                for h in (2 * pr, 2 * pr + 1):
                    attn_core(g, I, h,
                              lambda j: kTb[:, h, j * 128:(j + 1) * 128],
                              lambda qlo: qTb[:, h, I * 512 + qlo:(I + 1) * 512],
                              lambda j: Vb[:, j, h, :], SC_B, None, None, True, E, None, bO, bD, ecnt)
                attn_finish(g, l, I, pr, yT, rden, bO, bD, pr)
                g.reserved.discard(bO); g.reserved.discard(bD)
            if stage == 4 and g.dbg:
                for pr in range(4):
                    dump(g, yT[:, pr, :], 32768 + (I * 4 + pr) * 512, 'yT', bf=True)
            out_proj(g, I, yT, wo)
    P.barrier()


def moe_phase(g, l, stage):
    nc, P = g.nc, g.P
    mc = g.modc
    P.barrier()
    with ExitStack() as mo:
        sb = lambda name, shape, dtp=F32: g.sbt(mo, f"e{l}_" + name, shape, dtp)
        hT = sb("hT", [128, 8, S], BF16)
        norm_phase(g, g.nffn_d[l], mc[:, l, 32:40], mc[:, l, 24:32], hT, f"n{l}f_")
        allh = [('hT', t) for t in range(NT)]
        Gf = sb("Gf", [128, D])
        gate_tile(g, Gf, mc[:, l, 40:48], f"gf{l}_")
        gate = sb("gate", [128, NT, 32]); b1c = sb("b1c", [128, 16, 32])
        with ExitStack() as rt:
            sr = lambda name, shape, dtp=F32: g.sbt(rt, f"r{l}_" + name, shape, dtp)
            wrst = sr("wrst", [128, 8, 32]); wrb = sr("wrb", [128, 8, 32], BF16); brb = sr("brb", [128, 32])
            lg = sr("lg", [128, NT, 32]); ex = sr("ex", [128, NT, 32]); m8 = sr("m8", [128, NT, 8]); negm = sr("negm", [128, NT])
            den = sr("den", [128, NT]); gT = sr("gT", [32, S]); b2r = sr("b2r", [32, D]); b1r = sr("b1r", [32, 2048])
            P.op('sp', lambda e: e.dma_start(out=wrst[:], in_=g.wr_d[l].rearrange("(k p) e -> p k e", p=128)), [], ['wrst'], dma=1)
            P.op('sp', lambda e: e.dma_start(out=brb[:], in_=g.br_d[l].partition_broadcast(128)), [], ['brb'], dma=1)
            P.op('sp', lambda e: e.dma_start(out=b2r[:], in_=g.b2_d[l][:, :]), [], ['b2r'], dma=1)
            P.op('sp', lambda e: e.dma_start(out=b1r[:], in_=g.b1_d[l][:, :]), [], ['b1r'], dma=1)
            P.op('dve', lambda e: e.tensor_copy(out=wrb[:], in_=wrst[:]), ['wrst'], ['wrb'])
            for t in range(NT):
                b = nextps(g)
                P.mm([(g.ps[b][:, 0:32], hT[:, kc, t * 128:(t + 1) * 128], wrb[:, kc, :], kc == 0, kc == 7) for kc in range(8)],
                     ['wrb', ('hT', t)], [('ps', b)])
                P.op('dve', lambda e: e.tensor_tensor(out=lg[:, t, :], in0=g.ps[b][:, 0:32], in1=brb[:], op=ALU.add), [('ps', b), 'brb'], ['lg'])
                P.op('dve', lambda e: e.max(out=m8[:, t, :], in_=lg[:, t, :]), ['lg'], ['m8'])
            P.op('dve', lambda e: e.tensor_scalar(out=negm[:], in0=m8[:, :, 0], scalar1=-1.0, scalar2=None, op0=ALU.mult), ['m8'], ['negm'])
            for t in range(NT):
                P.op('act', lambda e: e.activation(out=ex[:, t, :], in_=lg[:, t, :], func=AF.Exp, bias=negm[:, t:t + 1], scale=1.0),
                     ['lg', 'negm'], ['ex'])
                P.op('dve', lambda e: e.scalar_tensor_tensor(out=gate[:, t, :], in0=lg[:, t, :], scalar=m8[:, t, 3:4], in1=ex[:, t, :],
                                                             op0=ALU.is_ge, op1=ALU.mult, accum_out=den[:, t:t + 1]), ['lg', 'm8', 'ex'], ['gate', 'den'])
            P.op('dve', lambda e: e.reciprocal(out=den[:], in_=den[:]), ['den'], ['den'])
            for t in range(NT):
                P.op('dve', lambda e: e.tensor_scalar(out=gate[:, t, :], in0=gate[:, t, :], scalar1=den[:, t:t + 1], scalar2=None, op0=ALU.mult),
                     ['gate', 'den'], ['gate'])
            P.op('dve', lambda e: e.tensor_tensor(out=b2r[:], in0=b2r[:], in1=Gf[0:32, :], op=ALU.mult), ['b2r', 'G'], ['b2r'])
            for t4 in range(4):
                b = nextps(g)
                P.tr([(g.ps[b][0:32, tt * 128:(tt + 1) * 128], gate[:, 4 * t4 + tt, :]) for tt in range(4)], g.identf[:],
                     ['gate', 'identf'], [('ps', b)])
                P.op('act', lambda e: e.activation(out=gT[:, t4 * 512:(t4 + 1) * 512], in_=g.ps[b][0:32, :], func=AF.Copy), [('ps', b)], ['gT'])
            for t in range(NT):
                for dh in range(2):
                    b = nextps(g)
                    P.mm([(g.ps[b][:, :], gT[:, t * 128:(t + 1) * 128], b2r[:, dh * 512:(dh + 1) * 512], True, True)], ['gT', 'b2r'], [('ps', b)])
                    P.op('dve', lambda e: e.tensor_tensor(out=g.xs[:, t, dh * 512:(dh + 1) * 512], in0=g.ps[b][:, :],
                                                          in1=g.xs[:, t, dh * 512:(dh + 1) * 512], op=ALU.add), [('ps', b), ('xs', t)], [('xs', t)])
            for c4 in range(4):
                b = nextps(g)
                P.tr([(g.ps[b][:, cc * 32:(cc + 1) * 32], b1r[:, (4 * c4 + cc) * 128:(4 * c4 + cc + 1) * 128]) for cc in range(4)],
                     g.identf[0:32, 0:32], ['b1r', 'identf'], [('ps', b)])
                P.op('act', lambda e: e.activation(out=b1c[:, 4 * c4:4 * c4 + 4, :], in_=g.ps[b][:, 0:128].rearrange("p (c e) -> p c e", c=4),
                                                   func=AF.Copy), [('ps', b)], ['b1c'])
            P.op('dve', lambda e: e.tensor_scalar(out=b1c[:, 8:16, :], in0=b1c[:, 8:16, :], scalar1=1.0, scalar2=None, op0=ALU.add), ['b1c'], ['b1c'])
            if stage == 5 and g.dbg:
                dump(g, gate[:].rearrange("p t e -> p (t e)"), 16384, 'gate')
        P.barrier()
        actT = sb("actT", [128, 8, S], BF16); w2b = sb("w2b", [128, 8, D], BF16)
        w1b = [sb(f"w1b{i}", [128, 8, 256], BF16) for i in range(2)]
        stg = [sb(f"ms{i}", [128, D]) for i in range(4)]
        gt = [sb(f"gt{i}", [128, 512]) for i in range(3)]; sg = [sb(f"sg{i}", [128, 512], BF16) for i in range(3)]
        lt = [sb(f"lt{i}", [128, 512]) for i in range(3)]; tt_ = [sb(f"tt{i}", [128, 512]) for i in range(3)]
        ns = [0]
        nexp = NE if stage != 5 else g.nexp_dbg

        def job_w1(e_, i, which):
            s_ = ns[0] % 4
            ns[0] += 1
            c0 = which * 1024 + i * 128
            src = g.w1_d[l, e_].rearrange("(k p) f -> p k f", p=128)[:, :, c0:c0 + 128]
            P.op('sp', lambda e: e.dma_start(out=stg[s_][:].rearrange("p (k f) -> p k f", k=8), in_=src), [], [('ms', s_)], dma=1)
            P.op('act', lambda e: e.activation(out=w1b[i % 2][:, :, which * 128:(which + 1) * 128],
                                               in_=stg[s_][:].rearrange("p (k f) -> p k f", k=8), func=AF.Copy),
                 [('ms', s_)], [('w1b', i % 2, which)])

        def job_w2(e_, fc):
            s_ = ns[0] % 4
            ns[0] += 1
            P.op('sp', lambda e: e.dma_start(out=stg[s_][:], in_=g.w2_d[l, e_][fc * 128:(fc + 1) * 128, :]), [], [('ms', s_)], dma=1)
            P.op('pool', lambda e: e.tensor_tensor(out=w2b[:, fc, :], in0=stg[s_][:], in1=Gf[:], op=ALU.mult), [('ms', s_), 'G'], [('w2b', fc)])

        k_ = 0
        pend = []

        def fin(i_, tg_, r3):
            tsl_ = slice(tg_ * 512, (tg_ + 1) * 512)
            P.op('dve', lambda e: e.scalar_tensor_tensor(out=actT[:, i_, tsl_], in0=lt[r3][:], scalar=8.0, in1=tt_[r3][:],
                                                         op0=ALU.min, op1=ALU.mult), [('lt', r3), ('tt', r3)], [('actT', i_, tg_)])

        for e_ in range(nexp):
            job_w1(e_, 0, 0); job_w1(e_, 0, 1)
            for i in range(8):
                if i + 1 < 8:
                    job_w1(e_, i + 1, 0); job_w1(e_, i + 1, 1)
                job_w2(e_, i)
                for tg in range(4):
                    tsl = slice(tg * 512, (tg + 1) * 512)
                    r_ = k_ % 3
                    k_ += 1
                    bG = nextps(g); bL = nextps(g)
                    P.mm([(g.ps[bG][:, :], w1b[i % 2][:, kc, 0:128], hT[:, kc, tsl], kc == 0, kc == 7) for kc in range(8)],
                         [('w1b', i % 2, 0)] + allh[4 * tg:4 * tg + 4], [('ps', bG)])
                    P.mm([(g.ps[bL][:, :], w1b[i % 2][:, kc, 128:256], hT[:, kc, tsl], kc == 0, kc == 7) for kc in range(8)],
                         [('w1b', i % 2, 1)] + allh[4 * tg:4 * tg + 4], [('ps', bL)])
                    P.op('dve', lambda e: e.tensor_scalar(out=gt[r_][:], in0=g.ps[bG][:, :], scalar1=b1c[:, i, e_:e_ + 1], scalar2=7.0,
                                                          op0=ALU.add, op1=ALU.min), [('ps', bG), 'b1c'], [('gt', r_)])
                    P.op('act', lambda e: e.activation(out=sg[r_][:], in_=gt[r_][:], func=AF.Sigmoid, scale=1.702), [('gt', r_)], [('sg', r_)])
                    P.op('dve', lambda e: e.tensor_scalar(out=lt[r_][:], in0=g.ps[bL][:, :], scalar1=b1c[:, 8 + i, e_:e_ + 1], scalar2=-6.0,
                                                          op0=ALU.add, op1=ALU.max), [('ps', bL), 'b1c'], [('lt', r_)])
                    P.op('pool', lambda e: e.tensor_tensor(out=tt_[r_][:], in0=gt[r_][:], in1=sg[r_][:], op=ALU.mult),
                         [('gt', r_), ('sg', r_)], [('tt', r_)])
                    if pend:
                        fin(*pend.pop(0))
                    pend.append((i, tg, r_))
            while pend:
                fin(*pend.pop(0))
            for t in range(NT):
                for dh in range(2):
                    b = nextps(g)
                    P.mm([(g.ps[b][:, :], actT[:, fc, t * 128:(t + 1) * 128], w2b[:, fc, dh * 512:(dh + 1) * 512], fc == 0, fc == 7) for fc in range(8)],
                         [('actT', fc, t // 4) for fc in range(8)] + [('w2b', fc) for fc in range(8)], [('ps', b)])
                    P.op('dve', lambda e: e.scalar_tensor_tensor(out=g.xs[:, t, dh * 512:(dh + 1) * 512], in0=g.ps[b][:, :],
                                                                 scalar=gate[:, t, e_:e_ + 1], in1=g.xs[:, t, dh * 512:(dh + 1) * 512],
                                                                 op0=ALU.mult, op1=ALU.add), [('ps', b), ('xs', t), 'gate'], [('xs', t)])
    P.barrier()
    if stage == 5 and g.dbg:
        for t in range(NT):
            dump(g, g.xs[:, t, :], t * 1024, ('xs', t))


def final_phase(g, stage):
    nc, P = g.nc, g.P
    P.barrier()
    with ExitStack() as ph:
        sb = lambda name, shape, dtp=F32: g.sbt(ph, "fin_" + name, shape, dtp)
        nfb = sb("nfb", [128, D]); ss = sb("ss", [128, NT]); rstd = sb("rstd", [128, NT]); junk = sb("junk", [128, D], BF16)
        ot = [sb(f"ot{i}", [128, D]) for i in range(2)]
        P.op('sp', lambda e: e.dma_start(out=nfb[:], in_=g.nfin_d.partition_broadcast(128)), [], ['nfb'], dma=1)
        for t in range(NT):
            P.op('act', lambda e: e.activation(out=junk[:], in_=g.xs[:, t, :], func=AF.Square, accum_out=ss[:, t:t + 1]),
                 [('xs', t)], ['fjunk', ('fss', t)])
        P.op('act', lambda e: e.activation(out=rstd[:], in_=ss[:], func=AF.Sqrt, scale=1.0 / D, bias=g.epsc[:, 0:1]),
             [('fss', t) for t in range(NT)] + ['epsc'], ['frstd'])
        P.op('dve', lambda e: e.reciprocal(out=rstd[:], in_=rstd[:]), ['frstd'], ['frstd'])
        yv = g.y_d.rearrange("(t p) d -> p t d", p=128)
        for t in range(NT):
            o = ot[t % 2]
            P.op('dve', lambda e: e.scalar_tensor_tensor(out=o[:], in0=g.xs[:, t, :], scalar=rstd[:, t:t + 1], in1=nfb[:],
                                                         op0=ALU.mult, op1=ALU.mult), [('xs', t), 'frstd', 'nfb'], [('ot', t % 2)])
            P.op('sp', lambda e: e.dma_start(out=yv[:, t, :], in_=o[:]), [('ot', t % 2)], [], dma=1)
        P.wait_all_dma('sp')


_CACHE = {}


def kernel(x, c, positions, rel_bias, norm_mix, w_ada, b_ada, w_in, q_norm, w_uq, kv_norm, w_ukv, w_out, norm_ffn,
           w_router, b_router, w1, b1, w2, b2, norm_final, _stage=99, _dbg=False, _cores=8):
    key = (_stage, _dbg)
    if key not in _CACHE:
        _CACHE[key] = build(_stage, _dbg)
    nc = _CACHE[key]
    f = lambda a: np.ascontiguousarray(np.asarray(a, dtype=np.float32))
    shared = {"rel_bias": f(rel_bias), "norm_mix": f(norm_mix), "w_ada": f(w_ada), "b_ada": f(b_ada), "w_in": f(w_in),
              "q_norm": f(q_norm), "w_uq": f(w_uq), "kv_norm": f(kv_norm), "w_ukv": f(w_ukv), "w_out": f(w_out),
              "norm_ffn": f(norm_ffn), "w_router": f(w_router), "b_router": f(b_router), "w1": f(w1), "b1": f(b1),
              "w2": f(w2), "b2": f(b2), "norm_final": f(norm_final)}
    shared.update(host_consts())
    x = f(x); c = f(c); positions = np.ascontiguousarray(np.asarray(positions, dtype=np.int32))
    in_maps = []
    for b in range(_cores):
        m = dict(shared)
        m["x"] = x[b]; m["c"] = c[b]; m["pos"] = positions[b]
        in_maps.append(m)
    res = run_bass_kernel_spmd(nc, in_maps, core_ids=list(range(_cores)))
    if _dbg:
        return res
    return np.stack([r["y"] for r in res.results], axis=0)
```

```python
import math
from contextlib import ExitStack
import numpy as np
import concourse.bass as bass
import concourse.mybir as mybir
from concourse.bass_utils import run_bass_kernel_spmd

F32 = mybir.dt.float32
BF16 = mybir.dt.bfloat16
I32 = mybir.dt.int32
U8 = mybir.dt.uint8
AF = mybir.ActivationFunctionType
ALU = mybir.AluOpType
AX = mybir.AxisListType

S = 2048
D = 1024
NT = 16
DEPTH = 2
NE = 32
EPS = 1e-6
TOPK = 256
NBIS = 16
BIG = 1.0e30
SC_A = 64 ** -0.5
SC_B = 96 ** -0.5


class Prog:
    NPOOL = 32

    def __init__(self, nc, es):
        self.nc = nc
        self.es = es
        self.eng = {'pe': nc.tensor, 'act': nc.scalar, 'dve': nc.vector, 'pool': nc.gpsimd, 'sp': nc.sync}
        self.sem = {e: es.enter_context(nc.semaphore("s_" + e)) for e in ('pe', 'act', 'dve', 'pool')}
        self.cnt = {e: 0 for e in self.sem}
        for k in range(self.NPOOL):
            self.sem[('d', k)] = es.enter_context(nc.semaphore(f"d_{k}"))
            self.cnt[('d', k)] = 0
        self.dnext = 0
        self.waited = {e: {} for e in self.eng}
        self.last_w = {}
        self.readers = {}
        self.nwaits = 0

    def _wait(self, e, tok):
        s, v = tok
        if s == 'pe' and e == 'pe':
            return
        if self.waited[e].get(s, 0) >= v:
            return
        self.eng[e].wait_ge(self.sem[s], v)
        self.waited[e][s] = v
        self.nwaits += 1

    def op(self, e, fn, reads=(), writes=(), dma=None):
        deps = {}
        for k in reads:
            t = self.last_w.get(k)
            if t is not None:
                deps[t[0]] = max(deps.get(t[0], 0), t[1])
        for k in writes:
            t = self.last_w.get(k)
            if t is not None:
                deps[t[0]] = max(deps.get(t[0], 0), t[1])
            for s, v in self.readers.get(k, {}).items():
                deps[s] = max(deps.get(s, 0), v)
        for s, v in deps.items():
            self._wait(e, (s, v))
        if dma is not None:
            assert e == 'sp'
            sk = ('d', self.dnext % self.NPOOL)
            self.dnext += 1
            if self.cnt[sk] > 0:
                self._wait(e, (sk, self.cnt[sk]))
            ins = fn(self.eng[e])
            ins.then_inc(self.sem[sk], 16)
            self.cnt[sk] += 16
            tok = (sk, self.cnt[sk])
        else:
            ins = fn(self.eng[e])
            ins.then_inc(self.sem[e], 1)
            self.cnt[e] += 1
            tok = (e, self.cnt[e])
        for k in writes:
            self.last_w[k] = tok
            self.readers[k] = {}
        for k in reads:
            r = self.readers.setdefault(k, {})
            r[tok[0]] = max(r.get(tok[0], 0), tok[1])
        return tok

    def wait_all_dma(self, e='sp'):
        for k in range(self.NPOOL):
            sk = ('d', k)
            if self.cnt[sk] > 0:
                self._wait(e, (sk, self.cnt[sk]))

    def barrier(self):
        for e in self.eng:
            for s in self.sem:
                if self.cnt[s] > 0:
                    self._wait(e, (s, self.cnt[s]))
        self.last_w = {}
        self.readers = {}

    def mm(self, items, reads=(), writes=()):
        def fn(pe):
            ins = None
            for (o, l, r, st, sp) in items:
                ins = pe.matmul(o, lhsT=l, rhs=r, start=st, stop=sp)
            return ins
        return self.op('pe', fn, reads, writes)

    def tr(self, items, ident, reads=(), writes=()):
        def fn(pe):
            ins = None
            for (o, i) in items:
                ins = pe.transpose(out=o, in_=i, identity=ident)
            return ins
        return self.op('pe', fn, reads, writes)


def t5_bucket_np(rel):
    nb = 16
    max_exact = 8
    ret = np.where(rel > 0, nb, 0)
    n = np.abs(rel)
    lg = np.log(np.maximum(n, 1).astype(np.float32) / np.float32(max_exact)).astype(np.float32)
    large = max_exact + (lg / np.float32(math.log(128 / max_exact)) * np.float32(nb - max_exact)).astype(np.float32).astype(np.int32)
    large = np.minimum(large, nb - 1)
    return ret + np.where(n < max_exact, n, large)


def host_consts():
    r = np.arange(128)[None, :]
    c = np.arange(128)[:, None]
    oh = np.zeros((32, 2, 128, 128), np.float32)
    for w in range(2):
        rel = (c - r) - 128 * w
        bk = t5_bucket_np(rel)
        for b in range(32):
            oh[b, w] = (bk == b)
    inv = (10000.0 ** (-np.arange(16, dtype=np.float32) / 16)).astype(np.float32)
    cols = np.zeros((128, 4), np.float32)
    for p in range(64, 96):
        i = (p - 64) % 16
        cols[p, 0] = inv[i]
        cols[p, 1] = -inv[i] if (p - 64) < 16 else inv[i]
    pw = np.zeros((128, NBIS), np.float32)
    pw[:] = 0.5 ** np.arange(1, NBIS + 1, dtype=np.float32)
    return {"oh": oh.reshape(32, 2 * 128 * 128), "ccols": cols, "pw2": pw}


class Ctx:
    pass


def build(stage=99, dbg=False):
    nc = bass.Bass("TRN2", target_bir_lowering=False)

    def dt(name, shape, dtp=F32, kind="ExternalInput"):
        return nc.dram_tensor(name, shape, dtp, kind=kind).ap()

    g = Ctx()
    g.nc = nc
    g.x_d = dt("x", [S, D]); g.c_d = dt("c", [D]); g.pos_d = dt("pos", [S], I32)
    g.relb_d = dt("rel_bias", [32, 8]); g.nmix_d = dt("norm_mix", [2, D]); g.wada_d = dt("w_ada", [2, D, 6 * D])
    g.bada_d = dt("b_ada", [2, 6 * D]); g.win_d = dt("w_in", [2, D, 2536]); g.qn_d = dt("q_norm", [2, 256])
    g.wuq_d = dt("w_uq", [2, 256, 768]); g.kvn_d = dt("kv_norm", [2, 128]); g.wukv_d = dt("w_ukv", [2, 128, 1024])
    g.wout_d = dt("w_out", [2, D, D]); g.nffn_d = dt("norm_ffn", [2, D]); g.wr_d = dt("w_router", [2, D, 32])
    g.br_d = dt("b_router", [2, 32]); g.w1_d = dt("w1", [2, 32, D, 2048]); g.b1_d = dt("b1", [2, 32, 2048])
    g.w2_d = dt("w2", [2, 32, D, D]); g.b2_d = dt("b2", [2, 32, D]); g.nfin_d = dt("norm_final", [D])
    g.oh_d = dt("oh", [32, 2 * 128 * 128]); g.ccols_d = dt("ccols", [128, 4]); g.pw2_d = dt("pw2", [128, NBIS])
    g.y_d = dt("y", [S, D], F32, "ExternalOutput")
    g.cqn_s = [dt(f"cqn_s{l}", [128, 2, S], BF16, "Internal") for l in range(2)]
    g.ckvn_s = [dt(f"ckvn_s{l}", [128, S], BF16, "Internal") for l in range(2)]
    g.krT_s = [dt(f"krT_s{l}", [32, S], BF16, "Internal") for l in range(2)]
    g.dbg = dbg
    if dbg:
        g.dbgf = dt("dbgf", [128, 32768], F32, "ExternalOutput")
        g.dbgb = dt("dbgb", [128, 65536], BF16, "ExternalOutput")

    es = ExitStack()
    P = Prog(nc, es)
    g.P = P

    def sbt(st, name, shape, dtp=F32):
        return st.enter_context(nc.sbuf_tensor("sb_" + name, shape, dtp))
    g.sbt = sbt

    g.xs = sbt(es, "xs", [128, NT, D])
    g.identb = sbt(es, "identb", [128, 128], BF16); g.identf = sbt(es, "identf", [128, 128])
    g.onesb = sbt(es, "onesb", [128, 128], BF16); g.onesf = sbt(es, "onesf", [128, 128])
    g.ccols = sbt(es, "ccols", [128, 4]); g.pw2 = sbt(es, "pw2", [128, NBIS]); g.epsc = sbt(es, "epsc", [128, 1])
    g.modc = sbt(es, "modc", [128, 2, 48])
    g.nbig = sbt(es, "nbig", [128, 1])
    g.ps = [es.enter_context(nc.psum_tensor(f"ps{i}", [128, 512], F32)) for i in range(8)]
    g.psn = 0
    import os
    g.nexp_dbg = int(os.environ.get('NEXP_DBG', NE))
    g.reserved = set()

    g.Tb = sbt(es, "t5_Tb", [128, 2, 8, 128], BF16)
    setup_phase(g)
    if stage >= 1:
        for l in range(DEPTH):
            mixer_phase(g, l, stage)
            if stage >= 5:
                moe_phase(g, l, stage)
            if stage < 9:
                break
    final_phase(g, stage)
    es.close()
    return nc


def nextps(g, reserve=False):
    while True:
        b = g.psn % 8
        g.psn += 1
        if b not in g.reserved:
            break
    if reserve:
        g.reserved.add(b)
    return b


def dump(g, ap, col, key, bf=False):
    if not g.dbg:
        return
    p, n = ap.shape[0], ap.shape[1]
    d = g.dbgb if bf else g.dbgf
    g.P.op('sp', lambda e: e.dma_start(out=d[0:p, col:col + n], in_=ap), [key] if not isinstance(key, list) else key, [], dma=1)


def setup_phase(g):
    nc, P = g.nc, g.P
    for t_, kn in ((g.identb, 'identb'), (g.identf, 'identf')):
        P.op('pool', lambda e: e.memset(t_[:], 1.0), [], [kn])
        P.op('pool', lambda e: e.affine_select(out=t_[:], in_=t_[:], pattern=[[-1, 128]], compare_op=ALU.is_equal,
                                               fill=0.0, base=0, channel_multiplier=1), [kn], [kn])
    P.op('pool', lambda e: e.memset(g.onesb[:], 1.0), [], ['onesb'])
    P.op('pool', lambda e: e.memset(g.onesf[:], 1.0), [], ['onesf'])
    P.op('pool', lambda e: e.memset(g.epsc[:], EPS), [], ['epsc'])
    P.op('pool', lambda e: e.memset(g.nbig[:], -30000.0), [], ['nbig'])
    P.op('sp', lambda e: e.dma_start(out=g.ccols[:], in_=g.ccols_d[:, :]), [], ['ccols'], dma=1)
    P.op('sp', lambda e: e.dma_start(out=g.pw2[:], in_=g.pw2_d[:, :]), [], ['pw2'], dma=1)
    xv = g.x_d.rearrange("(t p) d -> p t d", p=128)
    for q in range(4):
        P.op('sp', lambda e: e.dma_start(out=g.xs[:, 4 * q:4 * q + 4, :], in_=xv[:, 4 * q:4 * q + 4, :]), [],
             [('xs', t) for t in range(4 * q, 4 * q + 4)], dma=1)

    with ExitStack() as ph:
        sb = lambda name, shape, dtp=F32: g.sbt(ph, "su_" + name, shape, dtp)
        t5_tables(g, ph, "t5_")
        ccol = sb("ccol", [128, 8]); cond = sb("cond", [128, 8])
        wa = [sb(f"wa{i}", [128, 8, 512]) for i in range(4)]
        brow = sb("brow", [48, 2, 128]); badaT = sb("badaT", [128, 2, 48])
        P.op('sp', lambda e: e.dma_start(out=ccol[:], in_=g.c_d.rearrange("(k p) -> p k", p=128), allow_slow_non_contiguous=True),
             [], ['ccol'], dma=1)
        P.op('act', lambda e: e.activation(out=cond[:], in_=ccol[:], func=AF.Silu), ['ccol'], ['cond'])
        for l in range(2):
            P.op('sp', lambda e: e.dma_start(out=brow[:, l, :], in_=g.bada_d[l].rearrange("(j p) -> j p", p=128)), [], ['brow'], dma=1)
        for l in range(2):
            b = nextps(g)
            P.tr([(g.ps[b][:, 0:48], brow[:, l, :])], g.identf[0:48, 0:48], ['brow', 'identf'], [('ps', b)])
            P.op('dve', lambda e: e.tensor_copy(out=badaT[:, l, :], in_=g.ps[b][:, 0:48]), [('ps', b)], ['badaT'])
        n = 0
        for l in range(2):
            wv = g.wada_d[l].rearrange("(k p) n -> p k n", p=128)
            for cg in range(12):
                s_ = n % 4
                n += 1
                P.op('sp', lambda e: e.dma_start(out=wa[s_][:], in_=wv[:, :, cg * 512:(cg + 1) * 512]), [], [('wa', s_)], dma=1)
                b = nextps(g)
                items = []
                for jc in range(4):
                    for kc in range(8):
                        items.append((g.ps[b][:, jc:jc + 1], wa[s_][:, kc, jc * 128:(jc + 1) * 128], cond[:, kc:kc + 1], kc == 0, kc == 7))
                P.mm(items, [('wa', s_), 'cond'], [('ps', b)])
                P.op('dve', lambda e: e.tensor_tensor(out=g.modc[:, l, cg * 4:cg * 4 + 4], in0=g.ps[b][:, 0:4],
                                                      in1=badaT[:, l, cg * 4:cg * 4 + 4], op=ALU.add), [('ps', b), 'badaT'], ['modc'])
        dump(g, g.modc[:].rearrange("p l j -> p (l j)"), 0, 'modc')

    P.barrier()


def rope_tables(g, st, tag):
    nc, P = g.nc, g.P
    g.cosT = g.sbt(st, tag + "cosT", [128, S], BF16); g.sinT = g.sbt(st, tag + "sinT", [128, S], BF16)
    with ExitStack() as ph:
        sb = lambda name, shape, dtp=F32: g.sbt(ph, tag + name, shape, dtp)
        posi = sb("posi", [128, S], I32); posf = sb("posf", [128, S]); ang = sb("ang", [128, S])
        nn = posi[:].bitcast(F32)
        R_ = slice(64, 96)
        P.op('sp', lambda e: e.dma_start(out=posi[R_, :], in_=g.pos_d.partition_broadcast(32)), [], ['posi'], dma=1)
        P.op('dve', lambda e: e.tensor_copy(out=posf[R_, :], in_=posi[R_, :]), ['posi'], ['posf'])
        TWO_PI = 2.0 * math.pi
        HI = 6.28125
        LO = TWO_PI - HI
        MAGIC = 12582912.0
        for which, tab, shift in ((0, g.cosT, math.pi / 2), (1, g.sinT, 0.0)):
            P.op('dve', lambda e: e.tensor_scalar(out=ang[R_, :], in0=posf[R_, :], scalar1=g.ccols[R_, which:which + 1], scalar2=shift,
                                                  op0=ALU.mult, op1=ALU.add), ['posf', 'ccols'], ['ang'])
            P.op('dve', lambda e: e.tensor_scalar(out=nn[R_, :], in0=ang[R_, :], scalar1=1.0 / TWO_PI, scalar2=MAGIC,
                                                  op0=ALU.mult, op1=ALU.add), ['ang'], ['posi'])
            P.op('dve', lambda e: e.tensor_scalar(out=nn[R_, :], in0=nn[R_, :], scalar1=MAGIC, scalar2=None, op0=ALU.subtract), ['posi'], ['posi'])
            P.op('dve', lambda e: e.scalar_tensor_tensor(out=ang[R_, :], in0=nn[R_, :], scalar=-HI, in1=ang[R_, :],
                                                         op0=ALU.mult, op1=ALU.add), ['posi', 'ang'], ['ang'])
            P.op('dve', lambda e: e.scalar_tensor_tensor(out=ang[R_, :], in0=nn[R_, :], scalar=-LO, in1=ang[R_, :],
                                                         op0=ALU.mult, op1=ALU.add), ['posi', 'ang'], ['ang'])
            P.op('dve', lambda e: e.tensor_scalar(out=ang[R_, :], in0=ang[R_, :], scalar1=math.pi, scalar2=-math.pi,
                                                  op0=ALU.min, op1=ALU.max), ['ang'], ['ang'])
            P.op('act', lambda e: e.activation(out=tab[R_, :], in_=ang[R_, :], func=AF.Sin), ['ang'], ['ropetab'])
    P.barrier()


def t5_tables(g, ph, tag):
    nc, P = g.nc, g.P
    sb = lambda name, shape, dtp=F32: g.sbt(ph, tag + name, shape, dtp)
    ohp = sb("ohp", [32, 64 * 128]); rb = sb("rb", [32, 8]); rb15 = sb("rb15", [32, 8])
    P.op('sp', lambda e: e.dma_start(out=rb[:], in_=g.relb_d[:, :]), [], ['rb'], dma=1)
    P.op('sp', lambda e: e.dma_start(out=rb15[:], in_=g.relb_d[15].partition_broadcast(32)), [], ['rb15'], dma=1)
    P.op('dve', lambda e: e.tensor_tensor(out=rb[:], in0=rb[:], in1=rb15[:], op=ALU.subtract), ['rb', 'rb15'], ['rb'])
    P.op('dve', lambda e: e.tensor_scalar(out=rb[:], in0=rb[:], scalar1=1.0 / SC_A, scalar2=None, op0=ALU.mult), ['rb'], ['rb'])
    ohv = ohp[:].rearrange("b (c r) -> b c r", c=64)
    for w in range(2):
        for cq in range(2):
            o0 = (w * 128 + cq * 64) * 128
            P.op('sp', lambda e: e.dma_start(out=ohp[:], in_=g.oh_d[:, o0:o0 + 64 * 128]), [], ['ohp'], dma=1)
            b = nextps(g)
            items = []
            for c in range(64):
                items.append((g.ps[b][:, c * 8:(c + 1) * 8], ohv[:, c, :], rb[:], True, True))
            P.mm(items, ['ohp', 'rb'], [('ps', b)])
            P.op('dve', lambda e: e.tensor_copy(out=g.Tb[:, w, :, cq * 64:(cq + 1) * 64].rearrange("p h c -> p c h"),
                                                in_=g.ps[b][:].rearrange("p (c h) -> p c h", h=8)), [('ps', b)], ['Tb'])


def gate_tile(g, G, col, tag, key='G'):
    nc, P = g.nc, g.P
    with ExitStack() as ph:
        dg = [g.sbt(ph, f"{tag}dg{i}", [128, 128]) for i in range(2)]
        for half in range(2):
            b = nextps(g)
            for k4 in range(4):
                kc = half * 4 + k4
                d_ = dg[kc % 2]
                P.op('dve', lambda e: e.tensor_scalar(out=d_[:], in0=g.identf[:], scalar1=col[:, kc:kc + 1], scalar2=None, op0=ALU.mult),
                     ['identf', 'modc'], [('dg', kc % 2)])
                P.mm([(g.ps[b][:, k4 * 128:(k4 + 1) * 128], g.onesf[:], d_[:], True, True)], [('dg', kc % 2), 'onesf'], [('ps', b)])
            P.op('act', lambda e: e.activation(out=G[:, half * 512:(half + 1) * 512], in_=g.ps[b][:, :], func=AF.Copy), [('ps', b)], [key])
    P.barrier()


def norm_phase(g, nw_dram, sc_col, sh_col, hT, tag):
    nc, P = g.nc, g.P
    P.barrier()
    with ExitStack() as ph:
        sb = lambda name, shape, dtp=F32: g.sbt(ph, tag + name, shape, dtp)
        nwc = sb("nwc", [128, 8]); Ac = sb("Ac", [128, 8]); ss = sb("ss", [128, NT]); rstd = sb("rstd", [128, NT])
        junk = sb("junk", [128, D], BF16)
        xn = [sb(f"xn{i}", [128, D], BF16) for i in range(2)]
        P.op('sp', lambda e: e.dma_start(out=nwc[:], in_=nw_dram.rearrange("(k p) -> p k", p=128), allow_slow_non_contiguous=True),
             [], ['nwc'], dma=1)
        P.op('dve', lambda e: e.scalar_tensor_tensor(out=Ac[:], in0=sc_col, scalar=1.0, in1=nwc[:], op0=ALU.add, op1=ALU.mult),
             ['nwc', 'modc'], ['Ac'])
        for t in range(NT):
            P.op('act', lambda e: e.activation(out=junk[:], in_=g.xs[:, t, :], func=AF.Square, accum_out=ss[:, t:t + 1]),
                 [('xs', t)], ['njunk', ('nss', t)])
        P.op('act', lambda e: e.activation(out=rstd[:], in_=ss[:], func=AF.Sqrt, scale=1.0 / D, bias=g.epsc[:, 0:1]),
             [('nss', t) for t in range(NT)] + ['epsc'], ['nrstd'])
        P.op('dve', lambda e: e.reciprocal(out=rstd[:], in_=rstd[:]), ['nrstd'], ['nrstd'])
        for t in range(NT):
            x_ = xn[t % 2]
            P.op('dve', lambda e: e.tensor_scalar(out=x_[:], in0=g.xs[:, t, :], scalar1=rstd[:, t:t + 1], scalar2=None, op0=ALU.mult),
                 [('xs', t), 'nrstd'], [('xn', t % 2)])
            b = nextps(g)
            pv = g.ps[b][:].bitcast(BF16)
            P.tr([(pv[:, kc * 128:(kc + 1) * 128], x_[:, kc * 128:(kc + 1) * 128]) for kc in range(8)], g.identb[:],
                 [('xn', t % 2), 'identb'], [('ps', b)])
            for kc in range(8):
                o = hT[:, kc, t * 128:(t + 1) * 128]
                i = pv[:, kc * 128:(kc + 1) * 128]
                if kc % 2 == 0:
                    P.op('dve', lambda e: e.tensor_scalar(out=o, in0=i, scalar1=Ac[:, kc:kc + 1], scalar2=sh_col[:, kc:kc + 1],
                                                          op0=ALU.mult, op1=ALU.add), [('ps', b), 'Ac', 'modc'], [('hT', t)])
                else:
                    P.op('act', lambda e: e.activation(out=o, in_=i, func=AF.Identity, scale=Ac[:, kc:kc + 1], bias=sh_col[:, kc:kc + 1]),
                         [('ps', b), 'Ac', 'modc'], [('hT', t)])
    P.barrier()


def load_w(g, dram_ap, stg, wbf, skey, wkey, eng='act', cols=None):
    P = g.P
    P.op('sp', lambda e: e.dma_start(out=stg, in_=dram_ap), [], [skey], dma=1)
    if eng == 'act':
        P.op('act', lambda e: e.activation(out=wbf, in_=stg, func=AF.Copy), [skey], [wkey])
    else:
        P.op(eng, lambda e: e.tensor_copy(out=wbf, in_=stg), [skey], [wkey])


def evac(g, i, out, in_, reads, writes):
    if i % 2 == 0:
        g.P.op('act', lambda e: e.activation(out=out, in_=in_, func=AF.Copy), reads, writes)
    else:
        g.P.op('dve', lambda e: e.tensor_copy(out=out, in_=in_), reads, writes)


def mixer_phase(g, l, stage):
    mixer_a(g, l, stage)
    if stage == 2 or stage >= 4:
        mixer_b(g, l, stage)
    if stage == 4 and g.dbg:
        for t in range(NT):
            dump(g, g.xs[:, t, :], t * 1024, ('xs', t))


def make_getw(g, wv, stg, wbf):
    nw = [0]

    def getw(lo, n):
        s_ = nw[0] % 2
        nw[0] += 1
        load_w(g, wv[:, :, lo:lo + n], stg[s_][:, :, 0:n], wbf[s_][:, :, 0:n], ('stg', s_), ('wbf', s_),
               eng=('act' if s_ == 0 else 'dve'))
        return s_
    return getw


def mixer_a(g, l, stage):
    nc, P = g.nc, g.P
    P.barrier()
    mc = g.modc
    allh = [('hT', t) for t in range(NT)]
    with ExitStack() as mx:
        sbm = lambda name, shape, dtp=F32: g.sbt(mx, f"m{l}_" + name, shape, dtp)
        kTa = sbm("kTa", [128, 4, S], BF16); qTa = sbm("qTa", [128, 4, S], BF16); qiT = sbm("qiT", [128, 4, S], BF16)
        kiT = sbm("kiT", [128, S], BF16); va = sbm("va", [128, NT, 8, 64], BF16); widx = sbm("widx", [128, NT, 8])
        with ExitStack() as pj:
            sb = lambda name, shape, dtp=F32: g.sbt(pj, f"p{l}_" + name, shape, dtp)
            hT = sb("hT", [128, 8, S], BF16)
            norm_phase(g, g.nmix_d[l], mc[:, l, 8:16], mc[:, l, 0:8], hT, f"n{l}a_")
            if stage == 1:
                for kc in range(8):
                    dump(g, hT[:, kc, :], kc * 2048, allh, bf=True)
                return
            pa = ExitStack()
            sa = lambda name, shape, dtp=F32: g.sbt(pa, f"pa{l}_" + name, shape, dtp)
            stg = [sa(f"stg{i}", [128, 8, 128]) for i in range(2)]
            wbf = [sa(f"wbf{i}", [128, 8, 128], BF16) for i in range(2)]
            wki = sa("wki", [128, 8, 128], BF16)
            getw = make_getw(g, g.win_d[l].rearrange("(k p) n -> p k n", p=128), stg, wbf)

            def fm_proj(s_, c0, m, dst_fn, tag_):
                for tg in range(4):
                    b = nextps(g)
                    P.mm([(g.ps[b][0:m, :], wbf[s_][:, kc, c0:c0 + m], hT[:, kc, tg * 512:(tg + 1) * 512], kc == 0, kc == 7)
                          for kc in range(8)], [('wbf', s_)] + allh[4 * tg:4 * tg + 4], [('ps', b)])
                    evac(g, tg, dst_fn(tg), g.ps[b][0:m, :], [('ps', b)], [tag_])

            for (dst, c_lo, kn) in ((qTa, 0, 'qTa'), (kTa, 512, 'kTa'), (qiT, 1536, 'qiT')):
                for fch in range(4):
                    s_ = getw(c_lo + fch * 128, 128)
                    fm_proj(s_, 0, 128, lambda tg: dst[:, fch, tg * 512:(tg + 1) * 512], kn)
            for hp in range(4):
                s_ = getw(1024 + hp * 128, 128)
                for t in range(NT):
                    b = nextps(g)
                    P.mm([(g.ps[b][:, 0:128], hT[:, kc, t * 128:(t + 1) * 128], wbf[s_][:, kc, 0:128], kc == 0, kc == 7) for kc in range(8)],
                         [('wbf', s_), ('hT', t)], [('ps', b)])
                    evac(g, t, va[:, t, hp * 2:hp * 2 + 2, 0:64], g.ps[b][:, 0:128].rearrange("p (h d) -> p h d", h=2),
                         [('ps', b)], [('va', t)])
            s_ = getw(2048, 72)
            P.op('dve', lambda e: e.tensor_copy(out=wki[:, :, 0:64], in_=stg[s_][:, :, 0:64]), [('stg', s_)], ['wki'])
            P.op('dve', lambda e: e.tensor_copy(out=wki[:, :, 64:128], in_=stg[s_][:, :, 0:64]), [('stg', s_)], ['wki'])
            for tg in range(4):
                b = nextps(g)
                P.mm([(g.ps[b][:, :], wki[:, kc, :], hT[:, kc, tg * 512:(tg + 1) * 512], kc == 0, kc == 7) for kc in range(8)],
                     ['wki'] + allh[4 * tg:4 * tg + 4], [('ps', b)])
                evac(g, tg, kiT[:, tg * 512:(tg + 1) * 512], g.ps[b][:, :], [('ps', b)], ['kiT'])
            for t in range(NT):
                b = nextps(g)
                P.mm([(g.ps[b][:, 0:8], hT[:, kc, t * 128:(t + 1) * 128], wbf[s_][:, kc, 64:72], kc == 0, kc == 7) for kc in range(8)],
                     [('wbf', s_), ('hT', t)], [('ps', b)])
                evac(g, t, widx[:, t, :], g.ps[b][:, 0:8], [('ps', b)], ['widx'])
            P.barrier()
            pa.close()
            b_small_proj(g, l, hT)
            if stage == 2:
                o = 0
                for (tn, kn, nch) in ((qTa, 'qTa', 4), (kTa, 'kTa', 4), (qiT, 'qiT', 4)):
                    for c_ in range(nch):
                        dump(g, tn[:, c_, :], o, kn, bf=True); o += 2048
                dump(g, kiT[:], o, 'kiT', bf=True); o += 2048
                dump(g, va[:].rearrange("p t h d -> p (t h d)"), o, [('va', t) for t in range(NT)], bf=True); o += NT * 8 * 64
                dump(g, widx[:].rearrange("p t h -> p (t h)"), 0, 'widx')
        if stage == 2:
            return
        attn_a_phase(g, l, stage, kTa, qTa, qiT, kiT, va, widx)


def attn_core(g, I, h, lhs_k, rhs_q, v_ap, scale, bias_items, maskT, diag_zero, E, Pm, bO, bD, cnt, nqt=4):
    P = g.P
    W = nqt * 128
    jmax = nqt * I + nqt - 1
    hs = slice((h % 2) * 64, (h % 2) * 64 + 64)
    DEPTH_ = 2
    pend = []

    def front(j):
        qlo = max(0, j - nqt * I) * 128
        nq = W - qlo
        bS = nextps(g)
        items = [(g.ps[bS][:, qlo:W], lhs_k(j), rhs_q(qlo), True, True)]
        if bias_items is not None:
            items = [(g.ps[bS][:, qlo:W], lhs_k(j), rhs_q(qlo), True, False)]
            for i in (j, j + 1):
                if nqt * I <= i <= nqt * I + nqt - 1:
                    c0 = (i - nqt * I) * 128
                    items.append((g.ps[bS][:, c0:c0 + 128], bias_items(i - j), g.identb[:], False, False))
            items.append((g.ps[bS][:, qlo:W], g.identb[:], maskT[:, j, qlo:W], False, True))
        P.mm(items, ['attn_in', ('maskT', I % 2)] if maskT is not None else ['attn_in'], [('ps', bS)])
        k = cnt[0] % len(E)
        cnt[0] += 1
        P.op('act', lambda e: e.activation(out=E[k][:, 0:nq], in_=g.ps[bS][:, qlo:W], func=AF.Exp, scale=scale), [('ps', bS)], [('E', k)])
        if diag_zero and j >= nqt * I:
            P.op('pool', lambda e: e.memset(E[k][64:128, 0:64], 0.0), [], [('E', k)])
        rhs, rk = E[k][:, 0:nq], ('E', k)
        return (j, qlo, rhs, rk)

    def back(j, qlo, rhs, rk):
        P.mm([(g.ps[bO][hs, qlo:W], v_ap(j), rhs, j == 0, j == jmax),
              (g.ps[bD][hs, qlo:W], g.onesb[:, 0:64], rhs, j == 0, j == jmax)], [rk, 'attn_in', 'onesb'], [('ps', bO), ('ps', bD)])

    for j in range(jmax + 1):
        pend.append(front(j))
        if len(pend) > DEPTH_:
            back(*pend.pop(0))
    while pend:
        back(*pend.pop(0))


def attn_finish(g, l, I, npair_done, yT, rden, bO, bD, pr, W=512):
    P = g.P
    P.op('dve', lambda e: e.reciprocal(out=rden[:, 0:W], in_=g.ps[bD][:, 0:W]), [('ps', bD)], ['rden'])
    P.op('dve', lambda e: e.tensor_tensor(out=yT[:, pr, 0:W], in0=g.ps[bO][:, 0:W], in1=rden[:, 0:W], op=ALU.mult), [('ps', bO), 'rden'], ['yT'])


def load_wo(g, l, half, wo, wostg, Gm):
    P = g.P
    for pr in range(4):
        r0 = half * 512 + pr * 128
        P.op('sp', lambda e: e.dma_start(out=wostg[:], in_=g.wout_d[l][r0:r0 + 128, :]), [], ['wostg'], dma=1)
        P.op('dve' if pr % 2 else 'pool', lambda e: e.tensor_tensor(out=wo[:, pr, :], in0=wostg[:], in1=Gm[:], op=ALU.mult), ['wostg', 'G'], ['wo'])


def out_proj(g, I, yT, wo, nqt=4):
    P = g.P
    for tq in range(nqt):
        t = nqt * I + tq
        for dh in range(2):
            b = nextps(g)
            P.mm([(g.ps[b][:, :], yT[:, pr, tq * 128:(tq + 1) * 128], wo[:, pr, dh * 512:(dh + 1) * 512], pr == 0, pr == 3) for pr in range(4)],
                 ['yT', 'wo'], [('ps', b)])
            P.op('dve', lambda e: e.tensor_tensor(out=g.xs[:, t, dh * 512:(dh + 1) * 512], in0=g.ps[b][:, :],
                                                  in1=g.xs[:, t, dh * 512:(dh + 1) * 512], op=ALU.add), [('ps', b), ('xs', t)], [('xs', t)])


MASKNEG = 30000.0


def attn_a_phase(g, l, stage, kTa, qTa, qiT, kiT, va, widx):
    nc, P = g.nc, g.P
    P.barrier()
    with ExitStack() as at:
        sb = lambda name, shape, dtp=F32: g.sbt(at, f"a{l}_" + name, shape, dtp)
        sc = sb("sc", [128, 2, S]); Rr = [sb(f"R{i}", [128, 512], BF16) for i in range(3)]
        Dh = [sb(f"Dh{i}", [128, 8, 128], BF16) for i in range(2)]; selb = [sb(f"selb{i}", [128, S], BF16) for i in range(2)]
        maskT2 = sb("maskT", [128, 2, NT, 256], BF16)
        lo = sb("lo", [128, 2]); hi = sb("hi", [128, 2]); w0 = sb("w0", [128, 1]); steps = sb("steps", [128, NBIS])
        mid = sb("mid", [128, 2]); cntc = sb("cntc", [128, 2]); tmp = sb("tmp", [128, 2])
        E = [sb(f"E{i}", [128, 512], BF16) for i in range(4)]
        yT = sb("yT", [128, 4, 512], BF16); rden = sb("rden", [128, 512])
        wo = sb("wo", [128, 4, D], BF16)
        Gm = selb[1][:].bitcast(F32)
        wostg = selb[0][:].bitcast(F32)
        gate_tile(g, Gm, g.modc[:, l, 16:24], f"gm{l}a_", key=('selb', 1))
        for pr in range(4):
            r0 = pr * 128
            P.op('sp', lambda e: e.dma_start(out=wostg, in_=g.wout_d[l][r0:r0 + 128, :]), [], [('selb', 0)], dma=1)
            P.op('dve' if pr % 2 else 'pool', lambda e: e.tensor_tensor(out=wo[:, pr, :], in0=wostg, in1=Gm, op=ALU.mult),
                 [('selb', 0), ('selb', 1)], ['wo'])
        P.op('pool', lambda e: e.memset(maskT2[:], 0.0), [], [('maskT', 0), ('maskT', 1)])
        ecnt = [0]

        def static_pair(I, pair):
            maskT = maskT2[:, I % 2]
            for i in pair:
                qoff = (i - 2 * I) * 128
                P.op('pool', lambda e: e.memset(maskT[:, 0:i + 1, qoff:qoff + 128], 0.0), [], [('maskT', I % 2)])
                P.op('pool', lambda e: e.memset(maskT[64:128, i, qoff:qoff + 64], -MASKNEG), [], [('maskT', I % 2)])

        def scores_bisect(I, pair):
            for idx, i in enumerate(pair):
                ncols = (i + 1) * 128
                for h in range(8):
                    P.op('act', lambda e: e.activation(out=Dh[idx][:, h, :], in_=g.identb[:], func=AF.Copy, scale=widx[:, i, h:h + 1]),
                         ['identb', 'widx'], [('Dh', idx)])
                ncg = (ncols + 511) // 512
                for cg in range(ncg):
                    w = min(512, ncols - cg * 512)
                    bS = nextps(g, True)
                    pend = []
                    for h in range(8 + 2):
                        if h < 8:
                            hs = slice((h % 2) * 64, (h % 2) * 64 + 64)
                            bX = nextps(g)
                            r_ = h % 3
                            P.mm([(g.ps[bX][:, 0:w], qiT[hs, h // 2, i * 128:(i + 1) * 128], kiT[hs, cg * 512:cg * 512 + w], True, True)],
                                 ['qiT', 'kiT'], [('ps', bX)])
                            P.op('act', lambda e: e.activation(out=Rr[r_][:, 0:w], in_=g.ps[bX][:, 0:w], func=AF.Relu), [('ps', bX)], [('R', r_)])
                            pend.append((h, r_))
                        if h >= 2:
                            hh, rr = pend.pop(0)
                            P.mm([(g.ps[bS][:, 0:w], Dh[idx][:, hh, :], Rr[rr][:, 0:w], hh == 0, hh == 7)], [('Dh', idx), ('R', rr)], [('ps', bS)])
                    P.op('dve', lambda e: e.tensor_copy(out=sc[:, idx, cg * 512:cg * 512 + w], in_=g.ps[bS][:, 0:w]), [('ps', bS)], [('sc', idx)])
                    g.reserved.discard(bS)
                P.op('pool', lambda e: e.memset(sc[0:64, idx, ncols - 64:ncols], -BIG), [], [('sc', idx)])
                P.op('dve', lambda e: e.tensor_reduce(out=hi[:, idx:idx + 1], in_=sc[:, idx, 0:ncols], axis=AX.X, op=ALU.max), [('sc', idx)], ['hi'])
                P.op('dve', lambda e: e.tensor_reduce(out=lo[:, idx:idx + 1], in_=sc[:, idx, 0:ncols - 64], axis=AX.X, op=ALU.min), [('sc', idx)], ['lo'])
            P.op('dve', lambda e: e.tensor_tensor(out=hi[:], in0=hi[:], in1=lo[:], op=ALU.subtract), ['hi', 'lo'], ['hi'])
            P.op('dve', lambda e: e.tensor_tensor(out=w0[:], in0=hi[:, 0:1], in1=hi[:, 1:2], op=ALU.max), ['hi'], ['w0'])
            P.op('dve', lambda e: e.tensor_scalar(out=steps[:], in0=g.pw2[:], scalar1=w0[:, 0:1], scalar2=None, op0=ALU.mult),
                 ['pw2', 'w0'], ['steps'])
            P.op('dve', lambda e: e.tensor_scalar(out=mid[:], in0=lo[:], scalar1=steps[:, 0:1], scalar2=None, op0=ALU.add), ['lo', 'steps'], ['mid'])
            for k in range(NBIS):
                for idx, i in enumerate(pair):
                    ncols = (i + 1) * 128
                    P.op('dve', lambda e: e.tensor_scalar(out=selb[idx][:, 0:ncols], in0=sc[:, idx, 0:ncols], scalar1=mid[:, idx:idx + 1], scalar2=None,
                                                          op0=ALU.is_ge, op1=ALU.add, accum_out=cntc[:, idx:idx + 1]),
                         [('sc', idx), 'mid'], [('selb', idx), ('cntc', idx)])
                P.op('dve', lambda e: e.tensor_scalar(out=tmp[:], in0=cntc[:], scalar1=TOPK - 0.5, scalar2=steps[:, k:k + 1],
                                                      op0=ALU.is_ge, op1=ALU.mult), [('cntc', 0), ('cntc', 1), 'steps'], ['tmp'])
                if k + 1 < NBIS:
                    P.op('dve', lambda e: e.scalar_tensor_tensor(out=mid[:], in0=tmp[:], scalar=steps[:, k + 1:k + 2], in1=mid[:],
                                                                 op0=ALU.subtract, op1=ALU.add), ['tmp', 'steps', 'mid'], ['mid'])
                else:
                    P.op('dve', lambda e: e.scalar_tensor_tensor(out=lo[:], in0=mid[:], scalar=steps[:, k:k + 1], in1=tmp[:],
                                                                 op0=ALU.subtract, op1=ALU.add), ['tmp', 'steps', 'mid'], ['lo'])
            for idx, i in enumerate(pair):
                ncols = (i + 1) * 128
                P.op('dve', lambda e: e.tensor_scalar(out=selb[idx][:, 0:ncols], in0=sc[:, idx, 0:ncols], scalar1=lo[:, idx:idx + 1], scalar2=None,
                                                      op0=ALU.is_ge), [('sc', idx), 'lo'], [('selb', idx)])

        def finalize(I, pair):
            maskT = maskT2[:, I % 2]
            for idx, i in enumerate(pair):
                qoff = (i - 2 * I) * 128
                for jb in range(0, i + 1, 8):
                    n = min(8, i + 1 - jb)
                    b = nextps(g)
                    pv = g.ps[b][:].bitcast(BF16)
                    P.tr([(pv[:, jj * 128:(jj + 1) * 128], selb[idx][:, (jb + jj) * 128:(jb + jj + 1) * 128]) for jj in range(n)], g.identb[:],
                         [('selb', idx), 'identb'], [('ps', b)])
                    P.op('act', lambda e: e.activation(out=maskT[:, jb:jb + n, qoff:qoff + 128],
                                                       in_=pv[:, 0:n * 128].rearrange("p (j q) -> p j q", j=n), func=AF.Identity,
                                                       scale=MASKNEG, bias=g.nbig[:, 0:1]), [('ps', b), 'nbig'], [('maskT', I % 2)])

        def attend(I, prs):
            maskT = maskT2[:, I % 2]
            for pr in prs:
                bO = nextps(g, True); bD = nextps(g, True)
                for h in (2 * pr, 2 * pr + 1):
                    hs = slice((h % 2) * 64, (h % 2) * 64 + 64)
                    attn_core(g, I, h,
                              lambda j: kTa[hs, pr, j * 128:(j + 1) * 128],
                              lambda qlo: qTa[hs, pr, I * 256 + qlo:(I + 1) * 256],
                              lambda j: va[:, j, h, :], SC_A,
                              lambda w_: g.Tb[:, w_, h, :], maskT, False, E, None, bO, bD, ecnt, nqt=2)
                attn_finish(g, l, I, pr, yT, rden, bO, bD, pr, W=256)
                g.reserved.discard(bO); g.reserved.discard(bD)

        static_pair(0, (0, 1))
        scores_bisect(1, (2, 3)); finalize(1, (2, 3))
        for p in range(8):
            nxt = p + 2 < 8
            attend(p, (0, 1))
            if nxt:
                scores_bisect(p + 2, (2 * p + 4, 2 * p + 5))
            attend(p, (2, 3))
            if stage == 3:
                for pr in range(4):
                    dump(g, yT[:, pr, 0:256], 16384 + (p * 4 + pr) * 256, 'yT', bf=True)
            out_proj(g, p, yT, wo, nqt=2)
            if nxt:
                finalize(p + 2, (2 * p + 4, 2 * p + 5))
    P.barrier()


def b_small_proj(g, l, hT):
    nc, P = g.nc, g.P
    allh = [('hT', t) for t in range(NT)]
    R_ = slice(64, 96)
    P.barrier()
    with ExitStack() as pj:
        sb = lambda name, shape, dtp=F32: g.sbt(pj, f"pb{l}_" + name, shape, dtp)
        rope_tables(g, pj, f"rta{l}_")
        stg = [sb(f"stg{i}", [128, 8, 128]) for i in range(2)]
        wbf = [sb(f"wbf{i}", [128, 8, 128], BF16) for i in range(2)]
        wkr = sb("wkr", [128, 8, 96], BF16); wkrS = sb("wkrS", [128, 8, 96], BF16)
        qnc = sb("qnc", [128, 2]); kvnc = sb("kvnc", [128, 1])
        sq = [sb(f"sq{i}", [128, 512], BF16) for i in range(2)]
        cqr = sb("cqr", [128, 2, 512], BF16); r1 = sb("r1", [128, 512]); r2 = sb("r2", [128, 512])
        t1 = r1; t2 = r2
        oq = [sb("oq0", [128, 2, 512], BF16)] * 2
        okv = [sb("okv0", [128, 512], BF16)] * 2
        okr = [sb("okr0", [128, 512], BF16)] * 2
        getw = make_getw(g, g.win_d[l].rearrange("(k p) n -> p k n", p=128), stg, wbf)
        P.op('sp', lambda e: e.dma_start(out=qnc[:], in_=g.qn_d[l].rearrange("(k p) -> p k", p=128), allow_slow_non_contiguous=True),
             [], ['qnc'], dma=1)
        P.op('sp', lambda e: e.dma_start(out=kvnc[:], in_=g.kvn_d[l].rearrange("(k p) -> p k", p=128), allow_slow_non_contiguous=True),
             [], ['kvnc'], dma=1)
        sl = [getw(2120, 128), getw(2248, 128)]
        for tg in range(4):
            tsl = slice(tg * 512, (tg + 1) * 512)
            bs = nextps(g, True)
            for fch in range(2):
                b = nextps(g)
                P.mm([(g.ps[b][:, :], wbf[sl[fch]][:, kc, :], hT[:, kc, tsl], kc == 0, kc == 7)
                      for kc in range(8)], [('wbf', sl[fch])] + allh[4 * tg:4 * tg + 4], [('ps', b)])
                P.op('act', lambda e: e.activation(out=cqr[:, fch, :], in_=g.ps[b][:, :], func=AF.Copy), [('ps', b)], [('cqr', fch)])
                P.op('act', lambda e: e.activation(out=sq[fch][:], in_=g.ps[b][:, :], func=AF.Square), [('ps', b)], [('sq', fch)])
            P.mm([(g.ps[bs][:, :], g.onesb[:], sq[fch][:], fch == 0, fch == 1) for fch in range(2)],
                 [('sq', 0), ('sq', 1), 'onesb'], [('ps', bs)])
            g.reserved.discard(bs)
            P.op('act', lambda e: e.activation(out=r1[:], in_=g.ps[bs][:, :], func=AF.Sqrt, scale=1.0 / 256, bias=g.epsc[:, 0:1]),
                 [('ps', bs), 'epsc'], ['r1'])
            P.op('dve', lambda e: e.reciprocal(out=r2[:], in_=r1[:]), ['r1'], ['r2'])
            for fch in range(2):
                P.op('dve', lambda e: e.scalar_tensor_tensor(out=oq[tg % 2][:, fch, :], in0=cqr[:, fch, :],
                                                             scalar=qnc[:, fch:fch + 1], in1=r2[:], op0=ALU.mult, op1=ALU.mult),
                     [('cqr', fch), 'qnc', 'r2'], [('oq', 0)])
            P.op('sp', lambda e: e.dma_start(out=g.cqn_s[l][:, :, tsl], in_=oq[tg % 2][:]), [('oq', 0)], [], dma=1)
        s_ = getw(2376, 128)
        s2 = getw(2504, 32)
        P.op('pool', lambda e: e.memset(wkr[:], 0.0), [], ['wkr'])
        P.op('pool', lambda e: e.memset(wkrS[:], 0.0), [], ['wkrS'])
        P.op('dve', lambda e: e.tensor_copy(out=wkr[:, :, 64:96], in_=stg[s2][:, :, 0:32]), [('stg', s2)], ['wkr'])
        P.op('dve', lambda e: e.tensor_copy(out=wkrS[:, :, 64:80], in_=stg[s2][:, :, 16:32]), [('stg', s2)], ['wkrS'])
        P.op('dve', lambda e: e.tensor_copy(out=wkrS[:, :, 80:96], in_=stg[s2][:, :, 0:16]), [('stg', s2)], ['wkrS'])
        for tg in range(4):
            b = nextps(g); bs = nextps(g)
            tsl = slice(tg * 512, (tg + 1) * 512)
            P.mm([(g.ps[b][:, :], wbf[s_][:, kc, 0:128], hT[:, kc, tsl], kc == 0, kc == 7) for kc in range(8)],
                 [('wbf', s_)] + allh[4 * tg:4 * tg + 4], [('ps', b)])
            P.op('act', lambda e: e.activation(out=cqr[:, 0, :], in_=g.ps[b][:, :], func=AF.Copy), [('ps', b)], [('cqr', 0)])
            P.op('act', lambda e: e.activation(out=sq[0][:], in_=g.ps[b][:, :], func=AF.Square), [('ps', b)], [('sq', 0)])
            P.mm([(g.ps[bs][:, :], g.onesb[:], sq[0][:], True, True)], [('sq', 0), 'onesb'], [('ps', bs)])
            P.op('act', lambda e: e.activation(out=r1[:], in_=g.ps[bs][:, :], func=AF.Sqrt, scale=1.0 / 128, bias=g.epsc[:, 0:1]),
                 [('ps', bs), 'epsc'], ['r1'])
            P.op('dve', lambda e: e.reciprocal(out=r2[:], in_=r1[:]), ['r1'], ['r2'])
            P.op('dve', lambda e: e.scalar_tensor_tensor(out=okv[tg % 2][:], in0=cqr[:, 0, :], scalar=kvnc[:, 0:1], in1=r2[:],
                                                         op0=ALU.mult, op1=ALU.mult), [('cqr', 0), 'kvnc', 'r2'], [('okv', 0)])
            P.op('sp', lambda e: e.dma_start(out=g.ckvn_s[l][:, tsl], in_=okv[tg % 2][:]), [('okv', 0)], [], dma=1)
            ba = nextps(g); bb = nextps(g)
            P.mm([(g.ps[ba][0:96, :], wkr[:, kc, :], hT[:, kc, tsl], kc == 0, kc == 7) for kc in range(8)],
                 ['wkr'] + allh[4 * tg:4 * tg + 4], [('ps', ba)])
            P.mm([(g.ps[bb][0:96, :], wkrS[:, kc, :], hT[:, kc, tsl], kc == 0, kc == 7) for kc in range(8)],
                 ['wkrS'] + allh[4 * tg:4 * tg + 4], [('ps', bb)])
            P.op('dve', lambda e: e.tensor_tensor(out=t1[R_, :], in0=g.ps[ba][R_, :], in1=g.cosT[R_, tsl], op=ALU.mult),
                 [('ps', ba), 'ropetab'], ['r1'])
            P.op('dve', lambda e: e.tensor_tensor(out=t2[R_, :], in0=g.ps[bb][R_, :], in1=g.sinT[R_, tsl], op=ALU.mult),
                 [('ps', bb), 'ropetab'], ['r2'])
            P.op('pool', lambda e: e.tensor_tensor(out=okr[tg % 2][R_, :], in0=t1[R_, :], in1=t2[R_, :], op=ALU.add), ['r1', 'r2'], [('okr', 0)])
            P.op('sp', lambda e: e.dma_start(out=g.krT_s[l][:, tsl], in_=okr[tg % 2][R_, :]), [('okr', 0)], [], dma=1)
    P.barrier()


def mixer_b(g, l, stage):
    nc, P = g.nc, g.P
    P.barrier()
    R_ = slice(64, 96)
    with ExitStack() as mx:
        sbm = lambda name, shape, dtp=F32: g.sbt(mx, f"mb{l}_" + name, shape, dtp)
        cqn = sbm("cqn", [128, 2, S], BF16); ckvn = sbm("ckvn", [128, S], BF16); krT = sbm("krT", [128, S], BF16)
        rope_tables(g, mx, f"rt{l}_")
        P.op('sp', lambda e: e.dma_start(out=cqn[:], in_=g.cqn_s[l][:, :, :]), [], ['cqn'], dma=1)
        P.op('sp', lambda e: e.dma_start(out=ckvn[:], in_=g.ckvn_s[l][:, :]), [], ['ckvn'], dma=1)
        P.op('sp', lambda e: e.dma_start(out=krT[R_, :], in_=g.krT_s[l][:, :]), [], ['krT'], dma=1)
        if stage == 2:
            o = 12 * 2048 + 2048 + NT * 8 * 64
            for c_ in range(2):
                dump(g, cqn[:, c_, :], o, 'cqn', bf=True); o += 2048
            dump(g, ckvn[:], o, 'ckvn', bf=True); o += 2048
            dump(g, krT[R_, :], o, 'krT', bf=True); o += 2048
            return
        attn_b_phase(g, l, stage, cqn, ckvn, krT)


def attn_b_phase(g, l, stage, cqn, ckvn, krT):
    nc, P = g.nc, g.P
    P.barrier()
    R_ = slice(64, 96)
    with ExitStack() as at:
        sb = lambda name, shape, dtp=F32: g.sbt(at, f"b{l}_" + name, shape, dtp)
        qTb = sb("qTb", [96, 8, S], BF16); kTb = sb("kTb", [96, 8, S], BF16); Vb = sb("Vb", [128, NT, 8, 64], BF16)
        with ExitStack() as up:
            su = lambda name, shape, dtp=F32: g.sbt(up, f"u{l}_" + name, shape, dtp)
            wuqst = su("wuqst", [128, 2, 768]); wuq = su("wuq", [128, 2, 8, 96], BF16); wuqS = su("wuqS", [128, 2, 8, 96], BF16)
            wukvst = su("wukvst", [128, 1024]); wukv = su("wukv", [128, 1024], BF16)
            t1 = su("t1", [128, 512]); t2 = su("t2", [128, 512])
            P.op('sp', lambda e: e.dma_start(out=wuqst[:], in_=g.wuq_d[l].rearrange("(k p) n -> p k n", p=128)), [], ['wuqst'], dma=1)
            P.op('sp', lambda e: e.dma_start(out=wukvst[:], in_=g.wukv_d[l][:, :]), [], ['wukvst'], dma=1)
            wv4 = wuqst[:].rearrange("p k (h f) -> p k h f", h=8)
            P.op('act', lambda e: e.activation(out=wuq[:], in_=wv4, func=AF.Copy), ['wuqst'], ['wuq'])
            P.op('dve', lambda e: e.tensor_copy(out=wuqS[:, :, :, 0:64], in_=wv4[:, :, :, 0:64]), ['wuqst'], ['wuqS'])
            P.op('dve', lambda e: e.tensor_copy(out=wuqS[:, :, :, 64:80], in_=wv4[:, :, :, 80:96]), ['wuqst'], ['wuqS'])
            P.op('dve', lambda e: e.tensor_copy(out=wuqS[:, :, :, 80:96], in_=wv4[:, :, :, 64:80]), ['wuqst'], ['wuqS'])
            P.op('act', lambda e: e.activation(out=wukv[:], in_=wukvst[:], func=AF.Copy), ['wukvst'], ['wukv'])
            n = 0
            for h in range(8):
                for tg in range(4):
                    tsl = slice(tg * 512, (tg + 1) * 512)
                    bA = nextps(g); bB = nextps(g); bK = nextps(g)
                    P.mm([(g.ps[bA][0:96, :], wuq[:, kc, h, :], cqn[:, kc, tsl], kc == 0, kc == 1) for kc in range(2)], ['wuq', 'cqn'], [('ps', bA)])
                    P.mm([(g.ps[bB][0:96, :], wuqS[:, kc, h, :], cqn[:, kc, tsl], kc == 0, kc == 1) for kc in range(2)], ['wuqS', 'cqn'], [('ps', bB)])
                    P.mm([(g.ps[bK][0:64, :], wukv[:, h * 128:h * 128 + 64], ckvn[:, tsl], True, True)], ['wukv', 'ckvn'], [('ps', bK)])
                    P.op('act', lambda e: e.activation(out=qTb[0:64, h, tsl], in_=g.ps[bA][0:64, :], func=AF.Copy), [('ps', bA)], ['qTb'])
                    P.op('dve', lambda e: e.tensor_tensor(out=t1[R_, :], in0=g.ps[bA][R_, :], in1=g.cosT[R_, tsl], op=ALU.mult),
                         [('ps', bA), 'ropetab'], ['t1'])
                    P.op('dve', lambda e: e.tensor_tensor(out=t2[R_, :], in0=g.ps[bB][R_, :], in1=g.sinT[R_, tsl], op=ALU.mult),
                         [('ps', bB), 'ropetab'], ['t2'])
                    P.op('pool', lambda e: e.tensor_tensor(out=qTb[R_, h, tsl], in0=t1[R_, :], in1=t2[R_, :], op=ALU.add), ['t1', 't2'], ['qTb'])
                    evac(g, n, kTb[0:64, h, tsl], g.ps[bK][0:64, :], [('ps', bK)], ['kTb'])
                    n += 1
                P.op('pool', lambda e: e.tensor_copy(out=kTb[R_, h, :], in_=krT[R_, :]), ['krT'], ['kTb'])
            wvv = wukv[:].rearrange("p (h t d) -> p h t d", h=8, t=2)[:, :, 1, :]
            for t in range(NT):
                b = nextps(g)
                P.mm([(g.ps[b][:, :], ckvn[:, t * 128:(t + 1) * 128], wvv, True, True)], ['wukv', 'ckvn'], [('ps', b)])
                evac(g, t, Vb[:, t, :, :], g.ps[b][:, :].rearrange("p (h d) -> p h d", h=8), [('ps', b)], ['Vb'])
            if stage == 4 and g.dbg:
                for h in range(8):
                    dump(g, qTb[:, h, :], h * 2048, 'qTb', bf=True)
                    dump(g, kTb[:, h, :], 16384 + h * 2048, 'kTb', bf=True)
        P.barrier()
        Gm = sb("Gm", [128, D])
        gate_tile(g, Gm, g.modc[:, l, 16:24], f"gm{l}b_")
        E = [sb(f"E{i}", [128, 512], BF16) for i in range(4)]
        yT = sb("yT", [128, 4, 512], BF16); rden = sb("rden", [128, 512])
        wo = sb("wo", [128, 4, D], BF16); wostg = sb("wostg", [128, D])
        load_wo(g, l, 1, wo, wostg, Gm)
        ecnt = [0]
        for I in range(4):
            for pr in range(4):
                bO = nextps(g, True); bD = nextps(g, True)
                for h in (2 * pr, 2 * pr + 1):
                    attn_core(g, I, h,
                              lambda j: kTb[:, h, j * 128:(j + 1) * 128],
                              lambda qlo: qTb[:, h, I * 512 + qlo:(I + 1) * 512],
                              lambda j: Vb[:, j, h, :], SC_B, None, None, True, E, None, bO, bD, ecnt)
                attn_finish(g, l, I, pr, yT, rden, bO, bD, pr)
                g.reserved.discard(bO); g.reserved.discard(bD)
            if stage == 4 and g.dbg:
                for pr in range(4):
                    dump(g, yT[:, pr, :], 32768 + (I * 4 + pr) * 512, 'yT', bf=True)
            out_proj(g, I, yT, wo)
    P.barrier()


def moe_phase(g, l, stage):
    nc, P = g.nc, g.P
    mc = g.modc
    P.barrier()
    with ExitStack() as mo:
        sb = lambda name, shape, dtp=F32: g.sbt(mo, f"e{l}_" + name, shape, dtp)
        hT = sb("hT", [128, 8, S], BF16)
        norm_phase(g, g.nffn_d[l], mc[:, l, 32:40], mc[:, l, 24:32], hT, f"n{l}f_")
        allh = [('hT', t) for t in range(NT)]
        Gf = sb("Gf", [128, D])
        gate_tile(g, Gf, mc[:, l, 40:48], f"gf{l}_")
        gate = sb("gate", [128, NT, 32]); b1c = sb("b1c", [128, 16, 32])
        with ExitStack() as rt:
            sr = lambda name, shape, dtp=F32: g.sbt(rt, f"r{l}_" + name, shape, dtp)
            wrst = sr("wrst", [128, 8, 32]); wrb = sr("wrb", [128, 8, 32], BF16); brb = sr("brb", [128, 32])
            lg = sr("lg", [128, NT, 32]); ex = sr("ex", [128, NT, 32]); m8 = sr("m8", [128, NT, 8]); negm = sr("negm", [128, NT])
            den = sr("den", [128, NT]); gT = sr("gT", [32, S]); b2r = sr("b2r", [32, D]); b1r = sr("b1r", [32, 2048])
            P.op('sp', lambda e: e.dma_start(out=wrst[:], in_=g.wr_d[l].rearrange("(k p) e -> p k e", p=128)), [], ['wrst'], dma=1)
            P.op('sp', lambda e: e.dma_start(out=brb[:], in_=g.br_d[l].partition_broadcast(128)), [], ['brb'], dma=1)
            P.op('sp', lambda e: e.dma_start(out=b2r[:], in_=g.b2_d[l][:, :]), [], ['b2r'], dma=1)
            P.op('sp', lambda e: e.dma_start(out=b1r[:], in_=g.b1_d[l][:, :]), [], ['b1r'], dma=1)
            P.op('dve', lambda e: e.tensor_copy(out=wrb[:], in_=wrst[:]), ['wrst'], ['wrb'])
            for t in range(NT):
                b = nextps(g)
                P.mm([(g.ps[b][:, 0:32], hT[:, kc, t * 128:(t + 1) * 128], wrb[:, kc, :], kc == 0, kc == 7) for kc in range(8)],
                     ['wrb', ('hT', t)], [('ps', b)])
                P.op('dve', lambda e: e.tensor_tensor(out=lg[:, t, :], in0=g.ps[b][:, 0:32], in1=brb[:], op=ALU.add), [('ps', b), 'brb'], ['lg'])
                P.op('dve', lambda e: e.max(out=m8[:, t, :], in_=lg[:, t, :]), ['lg'], ['m8'])
            P.op('dve', lambda e: e.tensor_scalar(out=negm[:], in0=m8[:, :, 0], scalar1=-1.0, scalar2=None, op0=ALU.mult), ['m8'], ['negm'])
            for t in range(NT):
                P.op('act', lambda e: e.activation(out=ex[:, t, :], in_=lg[:, t, :], func=AF.Exp, bias=negm[:, t:t + 1], scale=1.0),
                     ['lg', 'negm'], ['ex'])
                P.op('dve', lambda e: e.scalar_tensor_tensor(out=gate[:, t, :], in0=lg[:, t, :], scalar=m8[:, t, 3:4], in1=ex[:, t, :],
                                                             op0=ALU.is_ge, op1=ALU.mult, accum_out=den[:, t:t + 1]), ['lg', 'm8', 'ex'], ['gate', 'den'])
            P.op('dve', lambda e: e.reciprocal(out=den[:], in_=den[:]), ['den'], ['den'])
            for t in range(NT):
                P.op('dve', lambda e: e.tensor_scalar(out=gate[:, t, :], in0=gate[:, t, :], scalar1=den[:, t:t + 1], scalar2=None, op0=ALU.mult),
                     ['gate', 'den'], ['gate'])
            P.op('dve', lambda e: e.tensor_tensor(out=b2r[:], in0=b2r[:], in1=Gf[0:32, :], op=ALU.mult), ['b2r', 'G'], ['b2r'])
            for t4 in range(4):
                b = nextps(g)
                P.tr([(g.ps[b][0:32, tt * 128:(tt + 1) * 128], gate[:, 4 * t4 + tt, :]) for tt in range(4)], g.identf[:],
                     ['gate', 'identf'], [('ps', b)])
                P.op('act', lambda e: e.activation(out=gT[:, t4 * 512:(t4 + 1) * 512], in_=g.ps[b][0:32, :], func=AF.Copy), [('ps', b)], ['gT'])
            for t in range(NT):
                for dh in range(2):
                    b = nextps(g)
                    P.mm([(g.ps[b][:, :], gT[:, t * 128:(t + 1) * 128], b2r[:, dh * 512:(dh + 1) * 512], True, True)], ['gT', 'b2r'], [('ps', b)])
                    P.op('dve', lambda e: e.tensor_tensor(out=g.xs[:, t, dh * 512:(dh + 1) * 512], in0=g.ps[b][:, :],
                                                          in1=g.xs[:, t, dh * 512:(dh + 1) * 512], op=ALU.add), [('ps', b), ('xs', t)], [('xs', t)])
            for c4 in range(4):
                b = nextps(g)
                P.tr([(g.ps[b][:, cc * 32:(cc + 1) * 32], b1r[:, (4 * c4 + cc) * 128:(4 * c4 + cc + 1) * 128]) for cc in range(4)],
                     g.identf[0:32, 0:32], ['b1r', 'identf'], [('ps', b)])
                P.op('act', lambda e: e.activation(out=b1c[:, 4 * c4:4 * c4 + 4, :], in_=g.ps[b][:, 0:128].rearrange("p (c e) -> p c e", c=4),
                                                   func=AF.Copy), [('ps', b)], ['b1c'])
            P.op('dve', lambda e: e.tensor_scalar(out=b1c[:, 8:16, :], in0=b1c[:, 8:16, :], scalar1=1.0, scalar2=None, op0=ALU.add), ['b1c'], ['b1c'])
            if stage == 5 and g.dbg:
                dump(g, gate[:].rearrange("p t e -> p (t e)"), 16384, 'gate')
        P.barrier()
        actT = sb("actT", [128, 8, S], BF16); w2b = sb("w2b", [128, 8, D], BF16)
        w1b = [sb(f"w1b{i}", [128, 8, 256], BF16) for i in range(2)]
        stg = [sb(f"ms{i}", [128, D]) for i in range(4)]
        gt = [sb(f"gt{i}", [128, 512]) for i in range(3)]; sg = [sb(f"sg{i}", [128, 512], BF16) for i in range(3)]
        lt = [sb(f"lt{i}", [128, 512]) for i in range(3)]; tt_ = [sb(f"tt{i}", [128, 512]) for i in range(3)]
        ns = [0]
        nexp = NE if stage != 5 else g.nexp_dbg

        def job_w1(e_, i, which):
            s_ = ns[0] % 4
            ns[0] += 1
            c0 = which * 1024 + i * 128
            src = g.w1_d[l, e_].rearrange("(k p) f -> p k f", p=128)[:, :, c0:c0 + 128]
            P.op('sp', lambda e: e.dma_start(out=stg[s_][:].rearrange("p (k f) -> p k f", k=8), in_=src), [], [('ms', s_)], dma=1)
            P.op('act', lambda e: e.activation(out=w1b[i % 2][:, :, which * 128:(which + 1) * 128],
                                               in_=stg[s_][:].rearrange("p (k f) -> p k f", k=8), func=AF.Copy),
                 [('ms', s_)], [('w1b', i % 2, which)])

        def job_w2(e_, fc):
            s_ = ns[0] % 4
            ns[0] += 1
            P.op('sp', lambda e: e.dma_start(out=stg[s_][:], in_=g.w2_d[l, e_][fc * 128:(fc + 1) * 128, :]), [], [('ms', s_)], dma=1)
            P.op('pool', lambda e: e.tensor_tensor(out=w2b[:, fc, :], in0=stg[s_][:], in1=Gf[:], op=ALU.mult), [('ms', s_), 'G'], [('w2b', fc)])

        k_ = 0
        pend = []

        def fin(i_, tg_, r3):
            tsl_ = slice(tg_ * 512, (tg_ + 1) * 512)
            P.op('dve', lambda e: e.scalar_tensor_tensor(out=actT[:, i_, tsl_], in0=lt[r3][:], scalar=8.0, in1=tt_[r3][:],
                                                         op0=ALU.min, op1=ALU.mult), [('lt', r3), ('tt', r3)], [('actT', i_, tg_)])

        for e_ in range(nexp):
            job_w1(e_, 0, 0); job_w1(e_, 0, 1)
            for i in range(8):
                if i + 1 < 8:
                    job_w1(e_, i + 1, 0); job_w1(e_, i + 1, 1)
                job_w2(e_, i)
                for tg in range(4):
                    tsl = slice(tg * 512, (tg + 1) * 512)
                    r_ = k_ % 3
                    k_ += 1
                    bG = nextps(g); bL = nextps(g)
                    P.mm([(g.ps[bG][:, :], w1b[i % 2][:, kc, 0:128], hT[:, kc, tsl], kc == 0, kc == 7) for kc in range(8)],
                         [('w1b', i % 2, 0)] + allh[4 * tg:4 * tg + 4], [('ps', bG)])
                    P.mm([(g.ps[bL][:, :], w1b[i % 2][:, kc, 128:256], hT[:, kc, tsl], kc == 0, kc == 7) for kc in range(8)],
                         [('w1b', i % 2, 1)] + allh[4 * tg:4 * tg + 4], [('ps', bL)])
                    P.op('dve', lambda e: e.tensor_scalar(out=gt[r_][:], in0=g.ps[bG][:, :], scalar1=b1c[:, i, e_:e_ + 1], scalar2=7.0,
                                                          op0=ALU.add, op1=ALU.min), [('ps', bG), 'b1c'], [('gt', r_)])
                    P.op('act', lambda e: e.activation(out=sg[r_][:], in_=gt[r_][:], func=AF.Sigmoid, scale=1.702), [('gt', r_)], [('sg', r_)])
                    P.op('dve', lambda e: e.tensor_scalar(out=lt[r_][:], in0=g.ps[bL][:, :], scalar1=b1c[:, 8 + i, e_:e_ + 1], scalar2=-6.0,
                                                          op0=ALU.add, op1=ALU.max), [('ps', bL), 'b1c'], [('lt', r_)])
                    P.op('pool', lambda e: e.tensor_tensor(out=tt_[r_][:], in0=gt[r_][:], in1=sg[r_][:], op=ALU.mult),
                         [('gt', r_), ('sg', r_)], [('tt', r_)])
                    if pend:
                        fin(*pend.pop(0))
                    pend.append((i, tg, r_))
            while pend:
                fin(*pend.pop(0))
            for t in range(NT):
                for dh in range(2):
                    b = nextps(g)
                    P.mm([(g.ps[b][:, :], actT[:, fc, t * 128:(t + 1) * 128], w2b[:, fc, dh * 512:(dh + 1) * 512], fc == 0, fc == 7) for fc in range(8)],
                         [('actT', fc, t // 4) for fc in range(8)] + [('w2b', fc) for fc in range(8)], [('ps', b)])
                    P.op('dve', lambda e: e.scalar_tensor_tensor(out=g.xs[:, t, dh * 512:(dh + 1) * 512], in0=g.ps[b][:, :],
                                                                 scalar=gate[:, t, e_:e_ + 1], in1=g.xs[:, t, dh * 512:(dh + 1) * 512],
                                                                 op0=ALU.mult, op1=ALU.add), [('ps', b), ('xs', t), 'gate'], [('xs', t)])
    P.barrier()
    if stage == 5 and g.dbg:
        for t in range(NT):
            dump(g, g.xs[:, t, :], t * 1024, ('xs', t))


def final_phase(g, stage):
    nc, P = g.nc, g.P
    P.barrier()
    with ExitStack() as ph:
        sb = lambda name, shape, dtp=F32: g.sbt(ph, "fin_" + name, shape, dtp)
        nfb = sb("nfb", [128, D]); ss = sb("ss", [128, NT]); rstd = sb("rstd", [128, NT]); junk = sb("junk", [128, D], BF16)
        ot = [sb(f"ot{i}", [128, D]) for i in range(2)]
        P.op('sp', lambda e: e.dma_start(out=nfb[:], in_=g.nfin_d.partition_broadcast(128)), [], ['nfb'], dma=1)
        for t in range(NT):
            P.op('act', lambda e: e.activation(out=junk[:], in_=g.xs[:, t, :], func=AF.Square, accum_out=ss[:, t:t + 1]),
                 [('xs', t)], ['fjunk', ('fss', t)])
        P.op('act', lambda e: e.activation(out=rstd[:], in_=ss[:], func=AF.Sqrt, scale=1.0 / D, bias=g.epsc[:, 0:1]),
             [('fss', t) for t in range(NT)] + ['epsc'], ['frstd'])
        P.op('dve', lambda e: e.reciprocal(out=rstd[:], in_=rstd[:]), ['frstd'], ['frstd'])
        yv = g.y_d.rearrange("(t p) d -> p t d", p=128)
        for t in range(NT):
            o = ot[t % 2]
            P.op('dve', lambda e: e.scalar_tensor_tensor(out=o[:], in0=g.xs[:, t, :], scalar=rstd[:, t:t + 1], in1=nfb[:],
                                                         op0=ALU.mult, op1=ALU.mult), [('xs', t), 'frstd', 'nfb'], [('ot', t % 2)])
            P.op('sp', lambda e: e.dma_start(out=yv[:, t, :], in_=o[:]), [('ot', t % 2)], [], dma=1)
        P.wait_all_dma('sp')


_CACHE = {}


def kernel(x, c, positions, rel_bias, norm_mix, w_ada, b_ada, w_in, q_norm, w_uq, kv_norm, w_ukv, w_out, norm_ffn,
           w_router, b_router, w1, b1, w2, b2, norm_final, _stage=99, _dbg=False, _cores=8):
    key = (_stage, _dbg)
    if key not in _CACHE:
        _CACHE[key] = build(_stage, _dbg)
    nc = _CACHE[key]
    f = lambda a: np.ascontiguousarray(np.asarray(a, dtype=np.float32))
    shared = {"rel_bias": f(rel_bias), "norm_mix": f(norm_mix), "w_ada": f(w_ada), "b_ada": f(b_ada), "w_in": f(w_in),
              "q_norm": f(q_norm), "w_uq": f(w_uq), "kv_norm": f(kv_norm), "w_ukv": f(w_ukv), "w_out": f(w_out),
              "norm_ffn": f(norm_ffn), "w_router": f(w_router), "b_router": f(b_router), "w1": f(w1), "b1": f(b1),
              "w2": f(w2), "b2": f(b2), "norm_final": f(norm_final)}
    shared.update(host_consts())
    x = f(x); c = f(c); positions = np.ascontiguousarray(np.asarray(positions, dtype=np.int32))
    in_maps = []
    for b in range(_cores):
        m = dict(shared)
        m["x"] = x[b]; m["c"] = c[b]; m["pos"] = positions[b]
        in_maps.append(m)
    res = run_bass_kernel_spmd(nc, in_maps, core_ids=list(range(_cores)))
    if _dbg:
        return res
    return np.stack([r["y"] for r in res.results], axis=0)
```
